# Optimizing a Trainium2 kernel written in Bass

```python
import jax
import jax.numpy as jnp
from jax import lax

D_MODEL = 1024
BATCH = 4
SEQ = 8192
DEPTH = 2

HEAD_DIM = 64
Q_BLOCK = 128
ROPE_THETA = 10000.0
NORM_EPS = 1e-6

A_HEADS = 8
A_KV_HEADS = 2
IDX_HEADS = 4
IDX_DIM = 64
DSA_TOPK = 256
B_HEADS = 8
B_KV_HEADS = 2
SWA_WINDOW = 128
C_HEADS = 8
C_KV_HEADS = 2
CMP_STRIDE = 16
CMP_LEN = 2 * CMP_STRIDE
CMP_HIDDEN = 128
SLC_BLOCK = 64
SLC_TOPN = 16
NSA_WINDOW = 512
D_HEADS = 8
Q_LORA = 256
KV_LORA = 128
NOPE_DIM = 64
ROPE_DIM = 32
V_DIM = 64

N_EVEN = (DEPTH + 1) // 2
N_ODD = DEPTH // 2

AB_SPLITS = (A_HEADS * HEAD_DIM, A_KV_HEADS * HEAD_DIM, A_KV_HEADS * HEAD_DIM, IDX_HEADS * IDX_DIM, IDX_DIM, IDX_HEADS, A_HEADS * HEAD_DIM, B_HEADS * HEAD_DIM, B_KV_HEADS * HEAD_DIM, B_KV_HEADS * HEAD_DIM, B_HEADS * HEAD_DIM)
AB_COLS = sum(AB_SPLITS)
AB_WIDTH = (A_HEADS + B_HEADS) * HEAD_DIM
CD_SPLITS = (C_HEADS * HEAD_DIM,) + (C_KV_HEADS * HEAD_DIM,) * 6 + (C_HEADS * 3, C_HEADS * HEAD_DIM, Q_LORA, KV_LORA, ROPE_DIM, D_HEADS * V_DIM)
CD_COLS = sum(CD_SPLITS)
CD_WIDTH = C_HEADS * HEAD_DIM + D_HEADS * V_DIM

kernel_name = 'hybrid_dsa_swa_nsa_mla_trunk'


def rms_norm(z, g):
    zf = z.astype(jnp.float32)
    y = zf * lax.rsqrt(jnp.mean(zf * zf, axis=-1, keepdims=True) + NORM_EPS)
    return (y * g.astype(jnp.float32)).astype(z.dtype)


def rope_tables(pos, dim):
    inv = jnp.power(jnp.float32(ROPE_THETA), -jnp.arange(0, dim, 2, dtype=jnp.float32) / dim)
    ang = pos.astype(jnp.float32)[:, None] * inv[None, :]
    return jnp.cos(ang), jnp.sin(ang)


def apply_rope(z, cos, sin):
    half = z.shape[-1] // 2
    z1 = z[..., :half].astype(jnp.float32)
    z2 = z[..., half:].astype(jnp.float32)
    c, s = cos[:, None, :], sin[:, None, :]
    return jnp.concatenate([z1 * c - z2 * s, z2 * c + z1 * s], axis=-1).astype(z.dtype)


def masked_softmax(s, mask):
    s = jnp.where(mask, s.astype(jnp.float32), -jnp.inf)
    m = jnp.max(s, axis=-1, keepdims=True)
    m = jnp.where(jnp.isfinite(m), m, 0.0)
    p = jnp.exp(s - m)
    return p / jnp.maximum(jnp.sum(p, axis=-1, keepdims=True), 1e-30)


def split_cols(y, sizes):
    offs, acc = [], 0
    for s in sizes[:-1]:
        acc += s
        offs.append(acc)
    return jnp.split(y, offs, axis=-1)


def dsa_attention(q, k, v, qi, ki, wi, k_sel):
    bsz, seq = q.shape[:2]
    grp = A_HEADS // A_KV_HEADS
    scale = HEAD_DIM ** -0.5
    idx_scale = (IDX_DIM * IDX_HEADS) ** -0.5
    key_pos = jnp.arange(seq)
    gather = jax.vmap(lambda zb, ib: zb[ib])

    def block(i):
        t0 = i * Q_BLOCK
        t = t0 + jnp.arange(Q_BLOCK)
        qb = lax.dynamic_slice_in_dim(q, t0, Q_BLOCK, 1).reshape(bsz, Q_BLOCK, A_KV_HEADS, grp, HEAD_DIM)
        qib = lax.dynamic_slice_in_dim(qi, t0, Q_BLOCK, 1)
        wib = lax.dynamic_slice_in_dim(wi, t0, Q_BLOCK, 1).astype(jnp.float32)
        logits = jnp.einsum('bqhd,bsd->bqhs', qib, ki).astype(jnp.float32)
        score = jnp.einsum('bqh,bqhs->bqs', wib, jax.nn.relu(logits)) * idx_scale
        causal = key_pos[None, :] <= t[:, None]
        score = jnp.where(causal[None], score, -jnp.inf)
        _, idx = lax.top_k(score, k_sel)
        valid = idx <= t[None, :, None]
        ks = gather(k, idx)
        vs = gather(v, idx)
        s = jnp.einsum('bqgrd,bqkgd->bqgrk', qb, ks) * scale
        p = masked_softmax(s, valid[:, :, None, None, :])
        o = jnp.einsum('bqgrk,bqkgd->bqgrd', p.astype(v.dtype), vs)
        return o.reshape(bsz, Q_BLOCK, A_HEADS * HEAD_DIM)

    out = lax.map(block, jnp.arange(seq // Q_BLOCK))
    return out.transpose(1, 0, 2, 3).reshape(bsz, seq, A_HEADS * HEAD_DIM)


def swa_sink_attention(q, k, v, sinks):
    bsz, seq = q.shape[:2]
    nb = seq // Q_BLOCK
    grp = B_HEADS // B_KV_HEADS
    scale = HEAD_DIM ** -0.5
    qb = q.reshape(bsz, nb, Q_BLOCK, B_KV_HEADS, grp, HEAD_DIM)

    def band(z):
        zp = jnp.pad(z, ((0, 0), (Q_BLOCK, 0), (0, 0), (0, 0))).reshape(bsz, nb + 1, Q_BLOCK, B_KV_HEADS, HEAD_DIM)
        return jnp.concatenate([zp[:, :-1], zp[:, 1:]], axis=2)

    kb, vb = band(k), band(v)
    s = jnp.einsum('bnqgrd,bnkgd->bngrqk', qb, kb).astype(jnp.float32) * scale
    qi = jnp.arange(Q_BLOCK)[None, :, None]
    kj = jnp.arange(2 * Q_BLOCK)[None, None, :]
    blk = jnp.arange(nb)[:, None, None]
    diff = qi + Q_BLOCK - kj
    key_abs = (blk - 1) * Q_BLOCK + kj
    mask = (diff >= 0) & (diff < SWA_WINDOW) & (key_abs >= 0)
    s = jnp.where(mask[None, :, None, None], s, -jnp.inf)
    sink = sinks.astype(jnp.float32).reshape(1, 1, B_KV_HEADS, grp, 1, 1)
    m = jnp.maximum(jnp.max(s, axis=-1, keepdims=True), sink)
    p = jnp.exp(s - m)
    p = p / (jnp.sum(p, axis=-1, keepdims=True) + jnp.exp(sink - m))
    o = jnp.einsum('bngrqk,bnkgd->bnqgrd', p.astype(v.dtype), vb)
    return o.reshape(bsz, seq, B_HEADS * HEAD_DIM)


def nsa_compress(z, pe, w1, w2):
    bsz, seq = z.shape[:2]
    ch = z.reshape(bsz, seq // CMP_STRIDE, CMP_STRIDE, C_KV_HEADS, HEAD_DIM)
    blocks = jnp.concatenate([ch[:, :-1], ch[:, 1:]], axis=2) + pe[None, None, :, None, :]
    flat = blocks.transpose(0, 1, 3, 2, 4).reshape(bsz, seq // CMP_STRIDE - 1, C_KV_HEADS, CMP_LEN * HEAD_DIM)
    return jax.nn.silu(flat @ w1) @ w2


def nsa_attention(q, kc, vc, ks, vs, kw, vw, gates, n_sel):
    bsz, seq = q.shape[:2]
    n_cmp = kc.shape[1]
    n_slc = seq // SLC_BLOCK
    grp = C_HEADS // C_KV_HEADS
    scale = HEAD_DIM ** -0.5
    ratio = SLC_BLOCK // CMP_STRIDE
    pad_back = ratio * n_slc + ratio - n_cmp - 1
    cmp_end = jnp.arange(n_cmp) * CMP_STRIDE + (CMP_LEN - 1)
    blk_ids = jnp.arange(n_slc)
    w_overlap = jnp.where(jnp.arange(ratio) == 0, 1.0, 2.0).astype(jnp.float32)
    ks_blk = ks.reshape(bsz, n_slc, SLC_BLOCK, C_KV_HEADS, HEAD_DIM).transpose(0, 3, 1, 2, 4)
    vs_blk = vs.reshape(bsz, n_slc, SLC_BLOCK, C_KV_HEADS, HEAD_DIM).transpose(0, 3, 1, 2, 4)
    kw_pad = jnp.pad(kw, ((0, 0), (NSA_WINDOW, 0), (0, 0), (0, 0)))
    vw_pad = jnp.pad(vw, ((0, 0), (NSA_WINDOW, 0), (0, 0), (0, 0)))
    gather = jax.vmap(jax.vmap(lambda zb, ib: zb[ib]))
    j_w = jnp.arange(Q_BLOCK + NSA_WINDOW)
    diff_w = jnp.arange(Q_BLOCK)[:, None] + NSA_WINDOW - j_w[None, :]

    def block(i):
        t0 = i * Q_BLOCK
        t = t0 + jnp.arange(Q_BLOCK)
        qb = lax.dynamic_slice_in_dim(q, t0, Q_BLOCK, 1).reshape(bsz, Q_BLOCK, C_KV_HEADS, grp, HEAD_DIM)
        gb = lax.dynamic_slice_in_dim(gates, t0, Q_BLOCK, 1).reshape(bsz, Q_BLOCK, C_KV_HEADS, grp, 3)
        s_c = jnp.einsum('bqgrd,bngd->bqgrn', qb, kc) * scale
        mask_c = cmp_end[None, :] <= t[:, None]
        p_c = masked_softmax(s_c, mask_c[None, :, None, None, :])
        o_c = jnp.einsum('bqgrn,bngd->bqgrd', p_c.astype(vc.dtype), vc)
        imp = jnp.pad(jnp.sum(p_c, axis=3), ((0, 0), (0, 0), (0, 0), (1, pad_back)))
        imp_s = jnp.sum(imp[..., :ratio * n_slc].reshape(bsz, Q_BLOCK, C_KV_HEADS, n_slc, ratio) * w_overlap, axis=-1) + imp[..., ratio::ratio]
        cur = t // SLC_BLOCK
        admiss = blk_ids[None, :] <= cur[:, None]
        forced = (blk_ids[None, :] == 0) | (blk_ids[None, :] >= cur[:, None] - 1)
        imp_s = jnp.where((admiss & forced)[None, :, None, :], jnp.inf, imp_s)
        imp_s = jnp.where(admiss[None, :, None, :], imp_s, -jnp.inf)
        _, sel = lax.top_k(imp_s, n_sel)
        sel_t = sel.transpose(0, 2, 1, 3)
        k_sel = gather(ks_blk, sel_t)
        v_sel = gather(vs_blk, sel_t).reshape(bsz, C_KV_HEADS, Q_BLOCK, n_sel * SLC_BLOCK, HEAD_DIM)
        s_s = jnp.einsum('bqgrd,bgqnld->bqgrnl', qb, k_sel) * scale
        s_s = s_s.reshape(bsz, Q_BLOCK, C_KV_HEADS, grp, n_sel * SLC_BLOCK)
        tok = sel[..., None] * SLC_BLOCK + jnp.arange(SLC_BLOCK)
        mask_s = (tok <= t[None, :, None, None, None]).reshape(bsz, Q_BLOCK, C_KV_HEADS, 1, n_sel * SLC_BLOCK)
        p_s = masked_softmax(s_s, mask_s)
        o_s = jnp.einsum('bqgrm,bgqmd->bqgrd', p_s.astype(vs.dtype), v_sel)
        kwb = lax.dynamic_slice_in_dim(kw_pad, t0, Q_BLOCK + NSA_WINDOW, 1)
        vwb = lax.dynamic_slice_in_dim(vw_pad, t0, Q_BLOCK + NSA_WINDOW, 1)
        s_w = jnp.einsum('bqgrd,bkgd->bqgrk', qb, kwb) * scale
        mask_w = (diff_w >= 0) & (diff_w < NSA_WINDOW) & ((t0 - NSA_WINDOW + j_w)[None, :] >= 0)
        p_w = masked_softmax(s_w, mask_w[None, :, None, None, :])
        o_w = jnp.einsum('bqgrk,bkgd->bqgrd', p_w.astype(vw.dtype), vwb)
        o = gb[..., 0:1] * o_c + gb[..., 1:2] * o_s + gb[..., 2:3] * o_w
        return o.reshape(bsz, Q_BLOCK, C_HEADS * HEAD_DIM)

    out = lax.map(block, jnp.arange(seq // Q_BLOCK))
    return out.transpose(1, 0, 2, 3).reshape(bsz, seq, C_HEADS * HEAD_DIM)


def mla_attention(q_nope, q_rope, k_nope, k_rope, v):
    bsz, seq = q_nope.shape[:2]
    scale = (NOPE_DIM + ROPE_DIM) ** -0.5
    key_pos = jnp.arange(seq)

    def block(i):
        t0 = i * Q_BLOCK
        t = t0 + jnp.arange(Q_BLOCK)
        qn = lax.dynamic_slice_in_dim(q_nope, t0, Q_BLOCK, 1)
        qr = lax.dynamic_slice_in_dim(q_rope, t0, Q_BLOCK, 1)
        s = (jnp.einsum('bqhd,bshd->bhqs', qn, k_nope) + jnp.einsum('bqhd,bsd->bhqs', qr, k_rope)) * scale
        mask = key_pos[None, :] <= t[:, None]
        p = masked_softmax(s, mask[None, None])
        o = jnp.einsum('bhqs,bshd->bqhd', p.astype(v.dtype), v)
        return o.reshape(bsz, Q_BLOCK, D_HEADS * V_DIM)

    out = lax.map(block, jnp.arange(seq // Q_BLOCK))
    return out.transpose(1, 0, 2, 3).reshape(bsz, seq, D_HEADS * V_DIM)


def layer_ab(x, norm_g, w_in, a_qk_norm, a_kidx_norm, b_qk_norm, b_sinks, w_out):
    bsz, seq, _ = x.shape
    y = rms_norm(x, norm_g) @ w_in
    qa, ka, va, qi, ki, wi, za, qb, kb, vb, zb = split_cols(y, AB_SPLITS)
    pos = jnp.arange(seq)
    cos, sin = rope_tables(pos, HEAD_DIM)
    icos, isin = rope_tables(pos, IDX_DIM)

    def heads(z, n):
        return z.reshape(bsz, seq, n, HEAD_DIM)

    qa = apply_rope(rms_norm(heads(qa, A_HEADS), a_qk_norm[0]), cos, sin)
    ka = apply_rope(rms_norm(heads(ka, A_KV_HEADS), a_qk_norm[1]), cos, sin)
    qi = apply_rope(qi.reshape(bsz, seq, IDX_HEADS, IDX_DIM), icos, isin)
    ki = apply_rope(rms_norm(ki, a_kidx_norm)[:, :, None, :], icos, isin)[:, :, 0]
    k_sel = min(DSA_TOPK, seq // 4)
    oa = dsa_attention(qa, ka, heads(va, A_KV_HEADS), qi, ki, wi, k_sel)
    qb = apply_rope(rms_norm(heads(qb, B_HEADS), b_qk_norm[0]), cos, sin)
    kb = apply_rope(rms_norm(heads(kb, B_KV_HEADS), b_qk_norm[1]), cos, sin)
    ob = swa_sink_attention(qb, kb, heads(vb, B_KV_HEADS), b_sinks)
    mix = jnp.concatenate([oa * jax.nn.silu(za), ob * jax.nn.silu(zb)], axis=-1)
    return x + mix @ w_out


def layer_cd(x, norm_g, w_in, c_q_norm, c_k_norm, c_cmp_pe, c_cmp_w1, c_cmp_w2, d_q_lat_norm, d_kv_lat_norm, d_w_uq, d_w_ukv, d_nope_norm, d_rope_norm, w_out):
    bsz, seq, _ = x.shape
    y = rms_norm(x, norm_g) @ w_in
    qc, kc, vc, ks, vs, kw, vw, gc, zc, cq, ckv, kr, zd = split_cols(y, CD_SPLITS)
    pos = jnp.arange(seq)
    cos, sin = rope_tables(pos, HEAD_DIM)

    def heads(z, n):
        return z.reshape(bsz, seq, n, HEAD_DIM)

    qc = apply_rope(rms_norm(heads(qc, C_HEADS), c_q_norm), cos, sin)
    kc = nsa_compress(heads(kc, C_KV_HEADS), c_cmp_pe[0], c_cmp_w1[0], c_cmp_w2[0])
    vc = nsa_compress(heads(vc, C_KV_HEADS), c_cmp_pe[1], c_cmp_w1[1], c_cmp_w2[1])
    n_cmp = kc.shape[1]
    ccos, csin = rope_tables(jnp.arange(n_cmp) * CMP_STRIDE + (CMP_LEN - 1), HEAD_DIM)
    kc = apply_rope(rms_norm(kc, c_k_norm[0]), ccos, csin)
    ks = apply_rope(rms_norm(heads(ks, C_KV_HEADS), c_k_norm[1]), cos, sin)
    kw = apply_rope(rms_norm(heads(kw, C_KV_HEADS), c_k_norm[2]), cos, sin)
    gates = jax.nn.sigmoid(gc.reshape(bsz, seq, C_HEADS, 3))
    n_sel = min(SLC_TOPN, seq // SLC_BLOCK)
    oc = nsa_attention(qc, kc, vc, ks, heads(vs, C_KV_HEADS), kw, heads(vw, C_KV_HEADS), gates, n_sel)
    q = (rms_norm(cq, d_q_lat_norm) @ d_w_uq).reshape(bsz, seq, D_HEADS, NOPE_DIM + ROPE_DIM)
    kv = (rms_norm(ckv, d_kv_lat_norm) @ d_w_ukv).reshape(bsz, seq, D_HEADS, NOPE_DIM + V_DIM)
    rcos, rsin = rope_tables(pos, ROPE_DIM)
    q_nope = rms_norm(q[..., :NOPE_DIM], d_nope_norm[0])
    q_rope = apply_rope(rms_norm(q[..., NOPE_DIM:], d_rope_norm[0]), rcos, rsin)
    k_nope = rms_norm(kv[..., :NOPE_DIM], d_nope_norm[1])
    k_rope = apply_rope(rms_norm(kr[:, :, None, :], d_rope_norm[1]), rcos, rsin)[:, :, 0]
    od = mla_attention(q_nope, q_rope, k_nope, k_rope, kv[..., NOPE_DIM:])
    mix = jnp.concatenate([oc * jax.nn.silu(zc), od * jax.nn.silu(zd)], axis=-1)
    return x + mix @ w_out


def setup_inputs(seed: int = 0) -> dict:
    key = jax.random.key(seed)
    k = jax.random.split(key, 22)
    f32 = jnp.float32

    def w(kk, shape, fan_in):
        return jax.random.normal(kk, shape, f32) * (fan_in ** -0.5)

    def gain(kk, shape):
        return 1.0 + 0.05 * jax.random.normal(kk, shape, f32)

    return {
        'x': jax.random.normal(k[0], (BATCH, SEQ, D_MODEL), f32),
        'ab_norm': gain(k[1], (N_EVEN, D_MODEL)),
        'ab_w_in': w(k[2], (N_EVEN, D_MODEL, AB_COLS), D_MODEL),
        'a_qk_norm': gain(k[3], (N_EVEN, 2, HEAD_DIM)),
        'a_kidx_norm': gain(k[4], (N_EVEN, IDX_DIM)),
        'b_qk_norm': gain(k[5], (N_EVEN, 2, HEAD_DIM)),
        'b_sinks': 0.5 * jax.random.normal(k[6], (N_EVEN, B_HEADS), f32),
        'ab_w_out': w(k[7], (N_EVEN, AB_WIDTH, D_MODEL), AB_WIDTH),
        'cd_norm': gain(k[8], (N_ODD, D_MODEL)),
        'cd_w_in': w(k[9], (N_ODD, D_MODEL, CD_COLS), D_MODEL),
        'c_q_norm': gain(k[10], (N_ODD, HEAD_DIM)),
        'c_k_norm': gain(k[11], (N_ODD, 3, HEAD_DIM)),
        'c_cmp_pe': 0.1 * jax.random.normal(k[12], (N_ODD, 2, CMP_LEN, HEAD_DIM), f32),
        'c_cmp_w1': w(k[13], (N_ODD, 2, CMP_LEN * HEAD_DIM, CMP_HIDDEN), CMP_LEN * HEAD_DIM),
        'c_cmp_w2': w(k[14], (N_ODD, 2, CMP_HIDDEN, HEAD_DIM), CMP_HIDDEN),
        'd_q_lat_norm': gain(k[15], (N_ODD, Q_LORA)),
        'd_kv_lat_norm': gain(k[16], (N_ODD, KV_LORA)),
        'd_w_uq': w(k[17], (N_ODD, Q_LORA, D_HEADS * (NOPE_DIM + ROPE_DIM)), Q_LORA),
        'd_w_ukv': w(k[18], (N_ODD, KV_LORA, D_HEADS * (NOPE_DIM + V_DIM)), KV_LORA),
        'd_nope_norm': gain(k[19], (N_ODD, 2, NOPE_DIM)),
        'd_rope_norm': gain(k[20], (N_ODD, 2, ROPE_DIM)),
        'cd_w_out': w(k[21], (N_ODD, CD_WIDTH, D_MODEL), CD_WIDTH),
    }


def reference(x, ab_norm, ab_w_in, a_qk_norm, a_kidx_norm, b_qk_norm, b_sinks, ab_w_out, cd_norm, cd_w_in, c_q_norm, c_k_norm, c_cmp_pe, c_cmp_w1, c_cmp_w2, d_q_lat_norm, d_kv_lat_norm, d_w_uq, d_w_ukv, d_nope_norm, d_rope_norm, cd_w_out):
    for layer in range(DEPTH):
        j = layer // 2
        if layer % 2 == 0:
            x = layer_ab(x, ab_norm[j], ab_w_in[j], a_qk_norm[j], a_kidx_norm[j], b_qk_norm[j], b_sinks[j], ab_w_out[j])
        else:
            x = layer_cd(x, cd_norm[j], cd_w_in[j], c_q_norm[j], c_k_norm[j], c_cmp_pe[j], c_cmp_w1[j], c_cmp_w2[j], d_q_lat_norm[j], d_kv_lat_norm[j], d_w_uq[j], d_w_ukv[j], d_nope_norm[j], d_rope_norm[j], cd_w_out[j])
    return x
```

```python
import numpy as np
import concourse.bass as bass
import concourse.mybir as mybir
from concourse.bass_utils import run_bass_kernel_spmd
from contextlib import ExitStack

F32 = mybir.dt.float32
BF16 = mybir.dt.bfloat16
ALU = mybir.AluOpType
AF = mybir.ActivationFunctionType
AX = mybir.AxisListType

SELF_SYNC = ('dve', 'act', 'pool')
NEG = -30000.0
S_ = 8192
NSB = 8
SB = 512


class Res:
    __slots__ = ('name', 'w', 'r')

    def __init__(self, name):
        self.name = name
        self.w = None
        self.r = {}


class Sched:
    ENG = ('pe', 'dve', 'act', 'pool', 'sp')

    def __init__(self, nc, es):
        self.nc = nc
        self.es = es
        self.sem = {e: es.enter_context(nc.semaphore('s_' + e)) for e in self.ENG}
        self.cnt = {e: 0 for e in self.ENG}
        self.clock = {e: {} for e in self.ENG}
        self.opclock = {}
        self.prog = {e: [] for e in self.ENG}
        self.dsem = {}
        self.waited = {e: set() for e in self.ENG}
        self.nwaits = 0

    def _deps(self, eng, reads, writes):
        waits = {}
        clk = self.clock[eng]

        def need(dep, selfok):
            src, n = dep
            if src == eng and not selfok:
                return
            if clk.get(src, 0) >= n:
                return
            if waits.get(src, 0) < n:
                waits[src] = n
        for r in reads:
            if r.w is not None:
                need(r.w, eng in SELF_SYNC)
        for w in writes:
            if w.w is not None:
                need(w.w, eng in SELF_SYNC)
            for src, n in w.r.items():
                need((src, n), eng in SELF_SYNC)
        for src, n in waits.items():
            self.prog[eng].append(('wait', src, n))
            self.nwaits += 1
            if src in self.ENG:
                self.waited[src].add(n)
            else:
                assert self.dsem[src][1] == n, ('partial DMA wait', src, n, self.dsem[src][1])
            oc = self.opclock[(src, n)]
            for k, v in oc.items():
                if clk.get(k, 0) < v:
                    clk[k] = v

    def _mark(self, ident, reads, writes):
        src, n = ident
        for r in reads:
            r.r[src] = n
        for w in writes:
            w.w = ident
            w.r = {}

    def op(self, eng, fn, reads=(), writes=()):
        self._deps(eng, reads, writes)
        self.cnt[eng] += 1
        n = self.cnt[eng]
        self.prog[eng].append(('op', fn, n))
        snap = dict(self.clock[eng])
        snap[eng] = n
        self.opclock[(eng, n)] = snap
        self._mark((eng, n), reads, writes)

    def dma(self, q, fn, reads=(), writes=(), sem=None):
        if sem not in self.dsem:
            self.dsem[sem] = [self.es.enter_context(self.nc.semaphore('d_' + sem)), 0]
        self._deps(q, reads, writes)
        self.dsem[sem][1] += 16
        n = self.dsem[sem][1]
        self.prog[q].append(('dma', fn, sem))
        snap = dict(self.clock[q])
        snap[sem] = n
        self.opclock[(sem, n)] = snap
        self._mark((sem, n), reads, writes)

    def barrier(self):
        tgt = [(e, self.cnt[e]) for e in self.ENG if self.cnt[e] > 0]
        dt_ = [(k, v[1]) for k, v in self.dsem.items() if v[1] > 0]
        for e in self.ENG:
            clk = self.clock[e]
            for src, n in tgt + dt_:
                if src == e or clk.get(src, 0) >= n:
                    continue
                self.prog[e].append(('wait', src, n))
                self.nwaits += 1
                if src in self.ENG:
                    self.waited[src].add(n)
                for k, v in self.opclock[(src, n)].items():
                    if clk.get(k, 0) < v:
                        clk[k] = v

    def finish(self, final_res=()):
        self._deps('sp', final_res, ())
        nc = self.nc
        incidx = {}
        for e in self.ENG:
            c = 0
            m = {}
            for n in range(1, self.cnt[e] + 1):
                if n in self.waited[e]:
                    c += 1
                    m[n] = c
            incidx[e] = m
        blk = self.es.enter_context(nc.Block())
        decos = {'pe': blk.tensor, 'dve': blk.vector, 'act': blk.scalar, 'pool': blk.gpsimd, 'sp': blk.sync}

        def make(e):
            prog = self.prog[e]

            def body(h):
                for it in prog:
                    if it[0] == 'wait':
                        src, n = it[1], it[2]
                        if src in self.ENG:
                            h.wait_ge(self.sem[src], incidx[src][n])
                        else:
                            h.wait_ge(self.dsem[src][0], n)
                    elif it[0] == 'op':
                        ins = it[1](h)
                        if it[2] in incidx[e]:
                            ins.then_inc(self.sem[e], 1)
                    else:
                        ins = it[1](h)
                        ins.then_inc(self.dsem[it[2]][0], 16)
            return body
        for e in self.ENG:
            if self.prog[e]:
                decos[e](make(e))


class KB:
    def __init__(self):
        self.nc = bass.Bass("TRN2", target_bir_lowering=False)
        self.es = ExitStack()
        self.S = None
        self.inputs = {}
        self.ndma = 0

    def start(self):
        self.S = Sched(self.nc, self.es)
        self.outer = self.es

    def push(self):
        self.stk = getattr(self, 'stk', [])
        self.stk.append(self.es)
        self.es = ExitStack()
        self.es.__enter__()

    def pop(self):
        self.S.barrier()
        self.es.__exit__(None, None, None)
        self.es = self.stk.pop()

    def din(self, name, shape, dt=F32):
        t = self.nc.dram_tensor(name, list(shape), dt, kind="ExternalInput").ap()
        self.inputs[name] = t
        return t

    def dout(self, name, shape, dt=F32):
        return self.nc.dram_tensor(name, list(shape), dt, kind="ExternalOutput").ap()

    def sb(self, name, shape, dt):
        self.nt = getattr(self, 'nt', 0) + 1
        name = "%s_%d" % (name, self.nt)
        t = self.es.enter_context(self.nc.sbuf_tensor(name, list(shape), dt))
        return t, Res(name)

    def ps(self, name, shape, dt):
        t = self.es.enter_context(self.nc.psum_tensor(name, list(shape), dt))
        return t, Res(name)

    def dma(self, out, in_, reads=(), writes=(), sem=None, q='sp', nonc=False):
        if sem is None:
            self.ndma += 1
            sem = 'x%d' % self.ndma
        if nonc:
            fn = lambda h: h.dma_start(out=out, in_=in_, allow_slow_non_contiguous=True)
        else:
            fn = lambda h: h.dma_start(out=out, in_=in_)
        self.S.dma(q, fn, reads=reads, writes=writes, sem=sem)

    def mm(self, out, lhsT, rhs, start, stop, reads, writes):
        self.S.op('pe', lambda h: h.matmul(out, lhsT=lhsT, rhs=rhs, start=start, stop=stop), reads=reads, writes=writes)

    def act(self, out, in_, func, reads, writes, scale=1.0, bias=None, accum_out=None):
        kw = {}
        if bias is not None:
            kw['bias'] = bias
        if accum_out is not None:
            kw['accum_out'] = accum_out
        self.S.op('act', lambda h: h.activation(out=out, in_=in_, func=func, scale=scale, **kw), reads=reads, writes=writes)

    def tt(self, eng, out, in0, in1, op, reads, writes):
        self.S.op(eng, lambda h: h.tensor_tensor(out=out, in0=in0, in1=in1, op=op), reads=reads, writes=writes)

    def ts(self, eng, out, in0, s1, s2, op0, op1, reads, writes, accum_out=None):
        if op1 is None:
            self.S.op(eng, lambda h: h.tensor_scalar(out=out, in0=in0, scalar1=s1, scalar2=None, op0=op0), reads=reads, writes=writes)
        elif accum_out is None:
            self.S.op(eng, lambda h: h.tensor_scalar(out=out, in0=in0, scalar1=s1, scalar2=s2, op0=op0, op1=op1), reads=reads, writes=writes)
        else:
            self.S.op(eng, lambda h: h.tensor_scalar(out=out, in0=in0, scalar1=s1, scalar2=s2, op0=op0, op1=op1, accum_out=accum_out), reads=reads, writes=writes)

    def reduce(self, out, in_, op, reads, writes):
        self.S.op('dve', lambda h: h.tensor_reduce(out=out, in_=in_, axis=AX.X, op=op), reads=reads, writes=writes)

    def stt(self, out, in0, scalar, in1, op0, op1, reads, writes):
        self.S.op('dve', lambda h: h.scalar_tensor_tensor(out=out, in0=in0, scalar=scalar, in1=in1, op0=op0, op1=op1), reads=reads, writes=writes)

    def copy(self, eng, out, in_, reads, writes):
        if eng == 'act':
            self.S.op(eng, lambda h: h.activation(out=out, in_=in_, func=AF.Copy), reads=reads, writes=writes)
        else:
            self.S.op(eng, lambda h: h.tensor_copy(out=out, in_=in_), reads=reads, writes=writes)

    def memset(self, eng, ap, val, writes):
        self.S.op(eng, lambda h: h.memset(ap, val), writes=writes)


def common_setup(kb):
    c = {}
    ident_d = kb.din("c_ident", [128, 128])
    blk_d = kb.din("c_blk64", [128, 128])
    rot_d = kb.din("c_rot64", [128, 128])
    ones_d = kb.din("c_ones", [128, 128])
    rot32_d = kb.din("c_rot32", [128, 128])
    stg, r_stg = kb.sb("cstg", [128, 5, 128], F32)
    for i, d in enumerate((ident_d, blk_d, rot_d, ones_d, rot32_d)):
        kb.dma(stg[:, i, :], d, writes=[r_stg], sem='cstg')
    cb, r_cb = kb.sb("cb", [128, 5, 128], BF16)
    kb.copy('dve', cb[:], stg[:], [r_stg], [r_cb])
    c['ident'] = cb[:, 0, :]
    c['blk'] = cb[:, 1, :]
    c['rot'] = cb[:, 2, :]
    c['ones'] = cb[:, 3, :]
    c['rot32'] = cb[:, 4, :]
    c['r_c'] = r_cb
    i4, r_i4 = kb.sb("i4", [128, 4, 128], BF16)
    kb.copy('dve', i4[:], stg[:, 0:1, :].to_broadcast([128, 4, 128]), [r_stg], [r_i4])
    c['i4'] = i4
    c['r_i4'] = r_i4
    c['bank'] = [kb.ps("bank%d" % i, [128, 512], F32) for i in range(7)]
    c['bankT'] = kb.ps("bankT", [128, 8, 128], BF16)
    c['xs'] = [kb.sb("xs%d" % i, [128, 1024], F32) for i in range(2)]
    c['xn'] = [kb.sb("xn%d" % i, [128, 1024], BF16) for i in range(2)]
    c['junk'] = kb.sb("junk", [128, 1024], BF16)
    c['st'] = [kb.sb("st%d" % i, [128, 4], F32) for i in range(2)]
    c['ntile'] = 0
    return c


def load_gain_T(kb, name, g_dram):
    t, r = kb.sb(name, [128, 8], F32)
    kb.dma(t[:], g_dram.rearrange("(k p) -> p k", p=128), writes=[r], nonc=True)
    return t, r


def load_col(kb, name, g_dram, n, reps):
    t, r = kb.sb(name, [n * reps, 1], F32)
    for i in range(reps):
        kb.dma(t[i * n:(i + 1) * n, :], g_dram.rearrange("(d o) -> d o", o=1), writes=[r], sem=name, nonc=True)
    return t, r


def norm_transpose_tile(kb, c, x_rows, gT, r_gT, xnT, r_xnT, col0):
    i = c['ntile'] % 2
    c['ntile'] += 1
    xs, r_xs = c['xs'][i]
    xn, r_xn = c['xn'][i]
    junk, r_junk = c['junk']
    st, r_st = c['st'][i]
    bT, r_bT = c['bankT']
    kb.dma(xs[:], x_rows, writes=[r_xs], sem='xs%d' % i)
    kb.act(junk[:], xs[:], AF.Square, [r_xs], [r_junk, r_st], accum_out=st[:, 0:1])
    kb.ts('dve', st[:, 1:2], st[:, 0:1], 1.0 / 1024, 1e-6, ALU.mult, ALU.add, [r_st], [r_st])
    kb.act(st[:, 2:3], st[:, 1:2], AF.Ln, [r_st], [r_st])
    kb.act(st[:, 3:4], st[:, 2:3], AF.Exp, [r_st], [r_st], scale=-0.5)
    kb.ts('dve', xn[:], xs[:], st[:, 3:4], None, ALU.mult, None, [r_xs, r_st], [r_xn])
    for k in range(8):
        kb.S.op('pe', lambda h, k=k: h.transpose(out=bT[:, k, :], in_=xn[:, k * 128:(k + 1) * 128], identity=c['ident']),
                reads=[r_xn, c['r_c']], writes=[r_bT])
    kb.tt('dve', xnT[:, :, col0:col0 + 128], bT[:], gT[:, :].unsqueeze(2).to_broadcast([128, 8, 128]), ALU.mult,
          [r_bT, r_gT], [r_xnT])


def proj_fm(kb, bank, r_bank, w, r_w, c0, M, xnT, r_xnT, t0, n):
    for k in range(8):
        kb.mm(bank[0:M, 0:n], w[:, k, c0:c0 + M], xnT[:, k, t0:t0 + n], k == 0, k == 7, [r_w, r_xnT], [r_bank])


def headnorm_rope(kb, c, bank, r_bank, n, gcol, r_gcol, cos, sin, r_cs, out, r_out, tmp, do_norm=True, P=64):
    zb, r_zb = tmp['zb']
    if do_norm:
        sq, r_sq = tmp['sq']
        rs, r_rs = tmp['rs']
        b2, r_b2 = c['bank'][2]
        kb.act(sq[0:P, 0:n], bank[0:P, 0:n], AF.Square, [r_bank], [r_sq])
        kb.mm(b2[0:P, 0:n], c['blk'][0:P, 0:P], sq[0:P, 0:n], True, True, [c['r_c'], r_sq], [r_b2])
        kb.act(rs[0:P, 0:n], b2[0:P, 0:n], AF.Ln, [r_b2], [r_rs], bias=tmp['eps'][0:P, 0:1])
        kb.act(rs[0:P, 0:n], rs[0:P, 0:n], AF.Exp, [r_rs], [r_rs], scale=-0.5)
        kb.stt(zb[0:P, 0:n], bank[0:P, 0:n], gcol[0:P, 0:1], rs[0:P, 0:n], ALU.mult, ALU.mult, [r_bank, r_gcol, r_rs], [r_zb])
    else:
        kb.copy('dve', zb[0:P, 0:n], bank[0:P, 0:n], [r_bank], [r_zb])
    b3, r_b3 = c['bank'][3]
    kb.mm(b3[0:P, 0:n], c['rot'][0:P, 0:P], zb[0:P, 0:n], True, True, [c['r_c'], r_zb], [r_b3])
    t1, r_t1 = tmp['t1']
    t2, r_t2 = tmp['t2']
    kb.tt('pool', t1[0:P, 0:n], zb[0:P, 0:n], cos, ALU.mult, [r_zb, r_cs], [r_t1])
    kb.tt('dve', t2[0:P, 0:n], b3[0:P, 0:n], sin, ALU.mult, [r_b3, r_cs], [r_t2])
    kb.tt('pool', out, t1[0:P, 0:n], t2[0:P, 0:n], ALU.add, [r_t1, r_t2], [r_out])


def hnr(kb, c, bank, r_bank, n, P, gcol, r_gcol, out, r_out, tmp, norm=None, rope=None):
    zb, r_zb = tmp['zb']
    dst = out if rope is None else zb[0:P, 0:n]
    r_dst = r_out if rope is None else r_zb
    if norm is not None:
        sq, r_sq = tmp['sq']
        rs, r_rs = tmp['rs']
        b2, r_b2 = c['bank'][2]
        kb.act(sq[0:P, 0:n], bank[0:P, 0:n], AF.Square, [r_bank], [r_sq])
        kb.mm(b2[0:P, 0:n], norm[0], sq[0:P, 0:n], True, True, [c['r_c'], r_sq], [r_b2])
        kb.act(rs[0:P, 0:n], b2[0:P, 0:n], AF.Ln, [r_b2], [r_rs], bias=tmp['eps'][0:P, 0:1], scale=norm[1])
        kb.act(rs[0:P, 0:n], rs[0:P, 0:n], AF.Exp, [r_rs], [r_rs], scale=-0.5)
        kb.stt(dst, bank[0:P, 0:n], gcol[0:P, 0:1], rs[0:P, 0:n], ALU.mult, ALU.mult, [r_bank, r_gcol, r_rs], [r_dst])
    else:
        kb.copy('dve', dst, bank[0:P, 0:n], [r_bank], [r_dst])
    if rope is None:
        return
    rot, cos, sin, r_cs = rope
    b3, r_b3 = c['bank'][3]
    kb.mm(b3[0:P, 0:n], rot, zb[0:P, 0:n], True, True, [c['r_c'], r_zb], [r_b3])
    t1, r_t1 = tmp['t1']
    t2, r_t2 = tmp['t2']
    kb.tt('pool', t1[0:P, 0:n], zb[0:P, 0:n], cos, ALU.mult, [r_zb, r_cs], [r_t1])
    kb.tt('dve', t2[0:P, 0:n], b3[0:P, 0:n], sin, ALU.mult, [r_b3, r_cs], [r_t2])
    kb.tt('pool', out, t1[0:P, 0:n], t2[0:P, 0:n], ALU.add, [r_t1, r_t2], [r_out])


def make_tmp(kb, n=512):
    tmp = {}
    tmp['zb'] = kb.sb("t_zb", [128, n], BF16)
    tmp['sq'] = kb.sb("t_sq", [128, n], BF16)
    tmp['sq2'] = kb.sb("t_sq2", [128, n], BF16)
    tmp['rs'] = kb.sb("t_rs", [128, n], F32)
    tmp['t1'] = kb.sb("t_t1", [128, n], F32)
    tmp['t2'] = kb.sb("t_t2", [128, n], F32)
    eps, r_eps = kb.sb("t_eps", [128, 1], F32)
    kb.memset('dve', eps[:], 1e-6, [r_eps])
    tmp['eps'] = eps
    return tmp


def attn_finalize(kb, c, ob, r_ob, zs_ap, r_zs, mix_ap, r_mix, fin):
    ln, r_ln = fin['ln']
    rec, r_rec = fin['rec']
    kb.act(ln[64:128, :], ob[64:128, :], AF.Ln, [r_ob], [r_ln])
    kb.act(rec[0:64, :], ln[64:128, :], AF.Exp, [r_ln], [r_rec], scale=-1.0)
    kb.tt('pool', rec[0:64, :].rearrange("p (h t) -> p h t", h=4), rec[0:64, :].rearrange("p (h t) -> p h t", h=4), zs_ap, ALU.mult,
          [r_rec, r_zs], [r_rec])
    kb.tt('dve', mix_ap, ob[0:64, :].rearrange("p (h t) -> p h t", h=4), rec[0:64, :].rearrange("p (h t) -> p h t", h=4), ALU.mult,
          [r_ob, r_rec], [r_mix])


def out_proj(kb, c, mixT, r_mix, nheads, wo, r_wo, prev, r_prev, osb, r_osb, out_rows, r_outd):
    for half in range(2):
        bk, r_bk = c['bank'][6]
        for h in range(nheads):
            kb.mm(bk[:, :], mixT[:, h, :], wo[:, h, half * 512:(half + 1) * 512], h == 0, h == nheads - 1, [r_mix, r_wo], [r_bk])
        kb.tt('dve', osb[:, half * 512:(half + 1) * 512], bk[:, :], prev[:, half * 512:(half + 1) * 512], ALU.add,
              [r_bk, r_prev], [r_osb])
    kb.dma(out_rows, osb[:], reads=[r_osb], writes=[r_outd], sem='osb_' + r_osb.name)


O_QA, O_KA, O_VA, O_QI, O_KI, O_WI, O_ZA, O_QB, O_KB, O_VB, O_ZB = 0, 512, 640, 768, 1024, 1088, 1092, 1604, 2116, 2244, 2372


def load_w_cols(kb, wt, r_w, dst0, w_dram, c0, n, sem):
    kb.dma(wt[:, :, dst0:dst0 + n], w_dram.rearrange("(k p) n -> p k n", p=128)[:, :, c0:c0 + n], writes=[r_w], sem=sem, q='pool')


def phase_B(kb, c, tmp, fin, d, first):
    S = kb.S
    wt, r_w = kb.sb("B_w", [128, 8, 1280], BF16)
    load_w_cols(kb, wt, r_w, 0, d['w_in'], O_QB, 512, 'B_w')
    load_w_cols(kb, wt, r_w, 512, d['w_in'], O_KB, 128, 'B_w')
    load_w_cols(kb, wt, r_w, 640, d['w_in'], O_VB, 128, 'B_w')
    load_w_cols(kb, wt, r_w, 768, d['w_in'], O_ZB, 512, 'B_w')
    wo, r_wo = kb.sb("B_wo", [64, 8, 1024], BF16)
    kb.dma(wo[:], d['w_out'][512:1024, :].rearrange("(h d) n -> d h n", d=64), writes=[r_wo], q='pool', sem='B_wo')
    gT, r_gT = load_gain_T(kb, "B_gT", d['norm'])
    gq, r_gq = load_col(kb, "B_gq", d['b_qk_norm'][0, :], 64, 1)
    gk, r_gk = load_col(kb, "B_gk", d['b_qk_norm'][1, :], 64, 1)
    sk, r_sk = kb.sb("B_sk", [1, 8], F32)
    kb.dma(sk[:], d['b_sinks'].rearrange("(o h) -> o h", o=1), writes=[r_sk])
    kb.act(sk[:], sk[:], AF.Exp, [r_sk], [r_sk])
    skr, r_skr = kb.sb("B_skr", [1, 8, 128], BF16)
    kb.copy('dve', skr[:], sk[:, :].unsqueeze(2).to_broadcast([1, 8, 128]), [r_sk], [r_skr])
    sel, r_sel = kb.sb("B_sel", [1, 128], BF16)
    kb.memset('dve', sel[:, 0:64], 0.0, [r_sel])
    kb.memset('dve', sel[:, 64:128], 1.0, [r_sel])
    mk, r_mk = kb.sb("B_mk", [128, 2, 4, 128], BF16)
    mk0, r_mk0 = kb.sb("B_mk0", [128, 4, 128], BF16)
    mstg, r_mstg = kb.sb("B_mstg", [128, 3, 128], F32)
    kb.dma(mstg[:], d['B_masks'].rearrange("a s t -> s a t"), writes=[r_mstg])
    kb.copy('dve', mk[:, 0, :, :], mstg[:, 0:1, :].to_broadcast([128, 4, 128]), [r_mstg], [r_mk])
    kb.copy('dve', mk[:, 1, :, :], mstg[:, 1:2, :].to_broadcast([128, 4, 128]), [r_mstg], [r_mk])
    kb.copy('dve', mk0[:], mstg[:, 2:3, :].to_broadcast([128, 4, 128]), [r_mstg], [r_mk0])

    xnT, r_xnT = kb.sb("B_xnT", [128, 8, 640], BF16)
    cs, r_cs = kb.sb("B_cs", [128, 2, 640], F32)
    QT, r_QT = kb.sb("B_QT", [64, 8, 512], BF16)
    KT, r_KT = kb.sb("B_KT", [64, 2, 640], BF16)
    V, r_V = kb.sb("B_V", [128, 5, 2, 128], BF16)
    kb.memset('pool', V[:, :, :, 64:128], 1.0, [r_V])
    zs, r_zs = kb.sb("B_zs", [64, 8, 512], BF16)
    PT = [kb.sb("B_PT%d" % i, [128, 512], BF16) for i in range(2)]
    mix, r_mix = kb.sb("B_mix", [64, 8, 128], BF16)
    prevs = [kb.sb("B_prev%d" % i, [128, 1024], F32) for i in range(2)]
    osbs = [kb.sb("B_osb%d" % i, [128, 1024], F32) for i in range(2)]
    r_outd = Res("outd")
    npt = 0
    import os
    STOP = int(os.environ.get('STOP', '9'))
    for m in range(NSB):
        if STOP < 1:
            break
        for j in range(5):
            norm_transpose_tile(kb, c, d['xq_halo'][m, j * 128:(j + 1) * 128, :], gT, r_gT, xnT, r_xnT, j * 128)
        kb.dma(cs[:], d['cs_halo'][m], writes=[r_cs], sem='B_cs')
        if STOP < 2:
            continue
        for g in range(2):
            for (t0, n) in ((0, 128), (128, 512)):
                bk, r_bk = c['bank'][g]
                proj_fm(kb, bk, r_bk, wt, r_w, 512 + g * 64, 64, xnT, r_xnT, t0, n)
                headnorm_rope(kb, c, bk, r_bk, n, gk, r_gk, cs[0:64, 0, t0:t0 + n], cs[0:64, 1, t0:t0 + n], r_cs, KT[:, g, t0:t0 + n], r_KT, tmp)
        for j in range(5):
            bk, r_bk = c['bank'][j % 2]
            for k in range(8):
                kb.mm(bk[:, 0:128], xnT[:, k, j * 128:(j + 1) * 128], wt[:, k, 640:768], k == 0, k == 7, [r_xnT, r_w], [r_bk])
            kb.copy('act', V[:, j, :, 0:64], bk[:, 0:128].rearrange("p (g d) -> p g d", g=2), [r_bk], [r_V])
        if STOP < 3:
            continue
        for h in range(8):
            bk, r_bk = c['bank'][h % 2]
            proj_fm(kb, bk, r_bk, wt, r_w, h * 64, 64, xnT, r_xnT, 128, 512)
            headnorm_rope(kb, c, bk, r_bk, 512, gq, r_gq, cs[0:64, 0, 128:640], cs[0:64, 1, 128:640], r_cs, QT[:, h, :], r_QT, tmp)
        for h in range(8):
            bk, r_bk = c['bank'][h % 2]
            proj_fm(kb, bk, r_bk, wt, r_w, 768 + h * 64, 64, xnT, r_xnT, 128, 512)
            kb.act(zs[:, h, :], bk[0:64, :], AF.Silu, [r_bk], [r_zs])
        if STOP < 4:
            continue
        for i in range(4):
            for g in range(2):
                ob, r_ob = c['bank'][6 if False else 5]
                for kk, jt in enumerate((i, i + 1)):
                    sbk, r_sbk = c['bank'][4 if kk == 0 else 3]
                    if kk == 0:
                        mrhs = mk0 if (m == 0 and i == 0) else None
                        mr = r_mk0 if (m == 0 and i == 0) else r_mk
                        rhs = mk0[:].rearrange("p a t -> p (a t)") if mrhs is not None else mk[:, 0, :, :].rearrange("p a t -> p (a t)")
                    else:
                        mr = r_mk
                        rhs = mk[:, 1, :, :].rearrange("p a t -> p (a t)")
                    kb.mm(sbk[:, :], c['ident'], rhs, True, False, [c['r_c'], mr], [r_sbk])
                    for hh in range(4):
                        h = 4 * g + hh
                        kb.mm(sbk[:, hh * 128:(hh + 1) * 128], KT[:, g, jt * 128:(jt + 1) * 128],
                              QT[:, h, i * 128:(i + 1) * 128], False, hh == 3, [r_KT, r_QT], [r_sbk])
                    pt, r_pt = PT[npt % 2]
                    npt += 1
                    kb.act(pt[:], sbk[:], AF.Exp, [r_sbk], [r_pt], scale=0.125)
                    kb.mm(ob[:, :], V[:, jt, g, :], pt[:], kk == 0, False, [r_V, r_pt], [r_ob])
                if STOP < 5:
                    continue
                kb.mm(ob[:, :], sel[:, :], skr[:, 4 * g:4 * g + 4, :].rearrange("p a t -> p (a t)"), False, True, [r_sel, r_skr], [r_ob])
                attn_finalize(kb, c, ob, r_ob, zs[:, 4 * g:4 * g + 4, i * 128:(i + 1) * 128], r_zs, mix[:, 4 * g:4 * g + 4, :], r_mix, fin)
            if STOP < 6:
                continue
            row0 = m * 512 + i * 128
            pv, r_pv = prevs[i % 2]
            osb, r_osb = osbs[i % 2]
            src = d['xq'] if first else d['out']
            kb.dma(pv[:], src[row0:row0 + 128, :], reads=[r_outd] if not first else [], writes=[r_pv], sem='B_prev%d' % (i % 2))
            out_proj(kb, c, mix, r_mix, 8, wo, r_wo, pv, r_pv, osb, r_osb, d['out'][row0:row0 + 128, :], r_outd)
    return r_outd


class WRing:
    def __init__(self, kb, name, nslots, ncols, zero=False):
        self.kb = kb
        self.name = name
        self.slots = [kb.sb("%s%d" % (name, i), [128, 8, ncols], BF16) for i in range(nslots)]
        self.n = 0
        if zero:
            for t, r in self.slots:
                kb.memset('pool', t[:], 0.0, [r])

    def load(self, w_dram, c0, n, dst=0):
        i = self.n % len(self.slots)
        self.n += 1
        t, r = self.slots[i]
        self.kb.dma(t[:, :, dst:dst + n], w_dram.rearrange("(k p) n -> p k n", p=128)[:, :, c0:c0 + n], writes=[r],
                    sem="%s%d" % (self.name, i), q='pool')
        return t, r


def phase_A(kb, c, tmp, fin, d, first):
    NCH = 2 * NSB
    NK = NCH * 512
    ka_d = kb.dout("A_ka_d", [128, NK], BF16)
    ki_d = kb.dout("A_ki_d", [64, NK], BF16)
    va_d = kb.dout("A_va_d", [128, NK // 128, 2, 128], BF16)
    r_kd = Res("A_kd")
    kast = [kb.sb("A_kast%d" % i, [128, 512], BF16) for i in range(2)]
    kist = [kb.sb("A_kist%d" % i, [64, 512], BF16) for i in range(2)]
    vast = [kb.sb("A_vast%d" % i, [128, 4, 2, 128], BF16) for i in range(2)]
    for t_, r_ in vast:
        kb.memset('pool', t_[:, :, :, 64:128], 1.0, [r_])
    wo, r_wo = kb.sb("A_wo", [64, 8, 1024], BF16)
    kb.dma(wo[:], d['w_out'][0:512, :].rearrange("(h d) n -> d h n", d=64), writes=[r_wo], q='pool', sem='A_wo')
    gT, r_gT = load_gain_T(kb, "A_gT", d['norm'])
    gq, r_gq = load_col(kb, "A_gq", d['a_qk_norm'][0, :], 64, 2)
    gk, r_gk = load_col(kb, "A_gk", d['a_qk_norm'][1, :], 64, 2)
    gi, r_gi = load_col(kb, "A_gi", d['a_kidx_norm'], 64, 1)
    xnT, r_xnT = kb.sb("A_xnT", [128, 8, 512], BF16)
    cs, r_cs = kb.sb("A_cs", [128, 2, 512], F32)
    ring = WRing(kb, "A_wr", 3, 128)
    ringL = WRing(kb, "A_wl", 2, 128, zero=True)
    ringR = WRing(kb, "A_wR", 2, 128, zero=True)
    wk, r_wk = kb.sb("A_wk", [128, 8, 320], BF16)
    load_w_cols(kb, wk, r_wk, 0, d['w_in'], O_KA, 128, 'A_wk')
    load_w_cols(kb, wk, r_wk, 128, d['w_in'], O_KI, 64, 'A_wk')
    load_w_cols(kb, wk, r_wk, 192, d['w_in'], O_VA, 128, 'A_wk')
    for ch in range(NCH):
        for j in range(4):
            norm_transpose_tile(kb, c, d['xb'][ch * 512 + j * 128:ch * 512 + (j + 1) * 128, :], gT, r_gT, xnT, r_xnT, j * 128)
        kb.dma(cs[:], d['cs_full'][:, :, ch * 512:(ch + 1) * 512], writes=[r_cs], sem='A_cs')
        bk, r_bk = c['bank'][0]
        proj_fm(kb, bk, r_bk, wk, r_wk, 0, 128, xnT, r_xnT, 0, 512)
        KaS, r_KaS = kast[ch % 2]
        KiS, r_KiS = kist[ch % 2]
        VaS, r_VaS = vast[ch % 2]
        headnorm_rope(kb, c, bk, r_bk, 512, gk, r_gk, cs[:, 0, :], cs[:, 1, :], r_cs, KaS[:, :], r_KaS, tmp, P=128)
        kb.dma(ka_d[:, ch * 512:(ch + 1) * 512], KaS[:, :], reads=[r_KaS], writes=[r_kd], sem='A_kast%d' % (ch % 2))
        bk, r_bk = c['bank'][1]
        proj_fm(kb, bk, r_bk, wk, r_wk, 128, 64, xnT, r_xnT, 0, 512)
        headnorm_rope(kb, c, bk, r_bk, 512, gi, r_gi, cs[0:64, 0, :], cs[0:64, 1, :], r_cs, KiS[:, :], r_KiS, tmp, P=64)
        kb.dma(ki_d[:, ch * 512:(ch + 1) * 512], KiS[:, :], reads=[r_KiS], writes=[r_kd], sem='A_kist%d' % (ch % 2))
        for j in range(4):
            bk, r_bk = c['bank'][j % 2]
            for k in range(8):
                kb.mm(bk[:, 0:128], xnT[:, k, j * 128:(j + 1) * 128], wk[:, k, 192:320], k == 0, k == 7, [r_xnT, r_wk], [r_bk])
            kb.copy('act', VaS[:, j, :, 0:64], bk[:, 0:128].rearrange("p (g d) -> p g d", g=2), [r_bk], [r_VaS])
        kb.dma(va_d[:, ch * 4:(ch + 1) * 4, :, :], VaS[:], reads=[r_VaS], writes=[r_kd], sem='A_vast%d' % (ch % 2))
    kb.S.barrier()
    kir = [kb.sb("A_kir%d" % i, [64, 512], BF16) for i in range(3)]
    kar = [kb.sb("A_kar%d" % i, [128, 512], BF16) for i in range(3)]
    var = [kb.sb("A_var%d" % i, [128, 4, 128], BF16) for i in range(3)]
    nld = [0, 0]
    QT, r_QT = kb.sb("A_QT", [128, 8, 512], BF16)
    QiT, r_QiT = kb.sb("A_QiT", [64, 4, 512], BF16)
    zs, r_zs = kb.sb("A_zs", [64, 8, 512], BF16)
    wis, r_wis = kb.sb("A_wis", [128, 4, 4], F32)
    score, r_score = kb.sb("A_score", [128, NK], F32)
    negm, r_negm = kb.sb("A_negm", [128, NK], BF16)
    caus, r_caus = kb.sb("A_caus", [128, 1024], F32)
    rls = [kb.sb("A_rl%d" % i, [128, 512], F32) for i in range(2)]
    bs, r_bs = kb.sb("A_bs", [128, 8], F32)
    PT = [kb.sb("A_PT%d" % i, [128, 512], BF16) for i in range(2)]
    mix, r_mix = kb.sb("A_mix", [64, 8, 128], BF16)
    pv, r_pv = kb.sb("A_prev", [128, 1024], F32)
    osb, r_osb = kb.sb("A_osb", [128, 1024], F32)
    r_outd = Res("outdA")
    npt = 0
    nrl = 0
    NIT = 20
    for m in range(NSB):
        nkeys = (2 * m + 2) * 512
        for j in range(4):
            norm_transpose_tile(kb, c, d['xq'][m * 512 + j * 128:m * 512 + (j + 1) * 128, :], gT, r_gT, xnT, r_xnT, j * 128)
        kb.dma(cs[:], d['cs_halo'][m, :, :, 128:640], writes=[r_cs], sem='A_cs')
        for h in range(8):
            rg = ringL if h < 4 else ringR
            wt_, r_wt = rg.load(d['w_in'], O_QA + h * 64, 64, dst=0 if h < 4 else 64)
            bk, r_bk = c['bank'][h % 2]
            proj_fm(kb, bk, r_bk, wt_, r_wt, 0, 128, xnT, r_xnT, 0, 512)
            headnorm_rope(kb, c, bk, r_bk, 512, gq, r_gq, cs[:, 0, :], cs[:, 1, :], r_cs, QT[:, h, :], r_QT, tmp, P=128)
        for h in range(4):
            wt_, r_wt = ring.load(d['w_in'], O_QI + h * 64, 64)
            bk, r_bk = c['bank'][h % 2]
            proj_fm(kb, bk, r_bk, wt_, r_wt, 0, 64, xnT, r_xnT, 0, 512)
            headnorm_rope(kb, c, bk, r_bk, 512, None, None, cs[0:64, 0, :], cs[0:64, 1, :], r_cs, QiT[:, h, :], r_QiT, tmp, do_norm=False, P=64)
        wt_, r_wt = ring.load(d['w_in'], O_WI, 4)
        for i in range(4):
            bk, r_bk = c['bank'][i % 2]
            for k in range(8):
                kb.mm(bk[:, 0:4], xnT[:, k, i * 128:(i + 1) * 128], wt_[:, k, 0:4], k == 0, k == 7, [r_xnT, r_wt], [r_bk])
            kb.copy('dve', wis[:, i, :], bk[:, 0:4], [r_bk], [r_wis])
        for h in range(8):
            wt_, r_wt = ring.load(d['w_in'], O_ZA + h * 64, 64)
            bk, r_bk = c['bank'][h % 2]
            proj_fm(kb, bk, r_bk, wt_, r_wt, 0, 64, xnT, r_xnT, 0, 512)
            kb.act(zs[:, h, :], bk[0:64, :], AF.Silu, [r_bk], [r_zs])
        for i in range(4):
            qs = slice(i * 128, (i + 1) * 128)
            for cc in range(nkeys // 512):
                ks = slice(cc * 512, (cc + 1) * 512)
                KiC, r_KiC = kir[nld[0] % 3]
                kb.dma(KiC[:, :], ki_d[:, ks], reads=[r_kd], writes=[r_KiC], sem='A_kir%d' % (nld[0] % 3))
                nld[0] += 1
                for h in range(4):
                    bk, r_bk = c['bank'][(cc * 4 + h) % 2]
                    kb.mm(bk[:, :], QiT[:, h, qs], KiC[:, :], True, True, [r_QiT, r_KiC], [r_bk])
                    rl, r_rl = rls[nrl % 2]
                    nrl += 1
                    kb.act(rl[:], bk[:, :], AF.Relu, [r_bk], [r_rl])
                    if h == 0:
                        kb.ts('dve', score[:, ks], rl[:], wis[:, i, 0:1], None, ALU.mult, None, [r_rl, r_wis], [r_score])
                    else:
                        kb.stt(score[:, ks], rl[:], wis[:, i, h:h + 1], score[:, ks], ALU.mult, ALU.add, [r_rl, r_wis, r_score], [r_score])
            sc = score[:, 0:nkeys]
            kb.reduce(bs[:, 5:6], sc, ALU.max, [r_score], [r_bs])
            kb.reduce(bs[:, 6:7], sc, ALU.min, [r_score], [r_bs])
            kb.dma(caus[:], d['A_caus'][i], writes=[r_caus], sem='A_caus')
            kb.tt('dve', score[:, nkeys - 1024:nkeys], score[:, nkeys - 1024:nkeys], caus[:], ALU.add, [r_score, r_caus], [r_score])
            kb.ts('dve', bs[:, 0:1], bs[:, 6:7], -1.0, None, ALU.add, None, [r_bs], [r_bs])
            kb.tt('dve', bs[:, 1:2], bs[:, 5:6], bs[:, 0:1], ALU.subtract, [r_bs], [r_bs])
            for it in range(NIT):
                ck = 2.0 ** -(it + 1)
                kb.stt(bs[:, 2:3], bs[:, 1:2], ck, bs[:, 0:1], ALU.mult, ALU.add, [r_bs], [r_bs])
                kb.ts('dve', negm[:, 0:nkeys], sc, bs[:, 2:3], None, ALU.is_gt, ALU.add, [r_score, r_bs], [r_negm, r_bs], accum_out=bs[:, 3:4])
                kb.ts('dve', bs[:, 4:5], bs[:, 3:4], 256.0, bs[:, 1:2], ALU.is_ge, ALU.mult, [r_bs], [r_bs])
                kb.stt(bs[:, 0:1], bs[:, 4:5], ck, bs[:, 0:1], ALU.mult, ALU.add, [r_bs], [r_bs])
            kb.ts('dve', negm[:, 0:nkeys], sc, bs[:, 0:1], NEG, ALU.is_le, ALU.mult, [r_score, r_bs], [r_negm])
            for g in range(2):
                ob, r_ob = c['bank'][5]
                nkt = nkeys // 128
                for kt in range(nkt):
                    if kt % 4 == 0:
                        KaC, r_KaC = kar[nld[1] % 3]
                        VaC, r_VaC = var[nld[1] % 3]
                        kb.dma(KaC[:, :], ka_d[:, kt * 128:kt * 128 + 512], reads=[r_kd], writes=[r_KaC], sem='A_kar%d' % (nld[1] % 3))
                        kb.dma(VaC[:], va_d[:, kt:kt + 4, g, :], reads=[r_kd], writes=[r_VaC], sem='A_var%d' % (nld[1] % 3))
                        nld[1] += 1
                    sbk, r_sbk = c['bank'][3 + (kt % 2)]
                    kb.mm(sbk[:, :], negm[:, kt * 128:(kt + 1) * 128], c['i4'][:].rearrange("p a t -> p (a t)"), True, False,
                          [r_negm, c['r_i4']], [r_sbk])
                    for hh in range(4):
                        kb.mm(sbk[:, hh * 128:(hh + 1) * 128], KaC[:, (kt % 4) * 128:(kt % 4 + 1) * 128], QT[:, 4 * g + hh, qs], False, hh == 3,
                              [r_KaC, r_QT], [r_sbk])
                    pt, r_pt = PT[npt % 2]
                    npt += 1
                    kb.act(pt[:], sbk[:], AF.Exp, [r_sbk], [r_pt], scale=0.125)
                    kb.mm(ob[:, :], VaC[:, kt % 4, :], pt[:], kt == 0, kt == nkt - 1, [r_VaC, r_pt], [r_ob])
                attn_finalize(kb, c, ob, r_ob, zs[:, 4 * g:4 * g + 4, qs], r_zs, mix[:, 4 * g:4 * g + 4, :], r_mix, fin)
            row0 = m * 512 + i * 128
            src = d['xq'] if first else d['out']
            kb.dma(pv[:], src[row0:row0 + 128, :], reads=[r_outd] if not first else [], writes=[r_pv], sem='A_prev')
            out_proj(kb, c, mix, r_mix, 8, wo, r_wo, pv, r_pv, osb, r_osb, d['out'][row0:row0 + 128, :], r_outd)
    return r_outd


def build_layer0(phases=('B',)):
    kb = KB()
    d = {}
    d['xb'] = kb.din("xb", [S_, 1024])
    d['xq'] = kb.din("xq", [NSB * SB, 1024])
    d['xq_halo'] = kb.din("xq_halo", [NSB, 640, 1024])
    d['cs_halo'] = kb.din("cs_halo", [NSB, 128, 2, 640])
    d['w_in'] = kb.din("w_in", [1024, 2884])
    d['w_out'] = kb.din("w_out", [1024, 1024])
    d['norm'] = kb.din("norm", [1024])
    d['b_qk_norm'] = kb.din("b_qk_norm", [2, 64])
    d['b_sinks'] = kb.din("b_sinks", [8])
    d['B_masks'] = kb.din("B_masks", [3, 128, 128])
    d['a_qk_norm'] = kb.din("a_qk_norm", [2, 64])
    d['a_kidx_norm'] = kb.din("a_kidx_norm", [64])
    d['cs_full'] = kb.din("cs_full", [128, 2, S_])
    d['A_caus'] = kb.din("A_caus", [4, 128, 1024])
    d['out'] = kb.dout("out", [NSB * SB, 1024])
    with kb.es:
        kb.start()
        c = common_setup(kb)
        tmp = make_tmp(kb)
        fin = {'ln': kb.sb("f_ln", [128, 512], F32), 'rec': kb.sb("f_rec", [128, 512], F32)}
        finals = []
        first = True
        for ph in phases:
            kb.push()
            if ph == 'B':
                finals.append(phase_B(kb, c, tmp, fin, d, first))
            elif ph == 'A':
                finals.append(phase_A(kb, c, tmp, fin, d, first))
            kb.pop()
            first = False
        kb.S.finish(finals)
    return kb


O_QC, O_KC, O_VC, O_KS, O_VS, O_KW, O_VW, O_GC, O_ZC, O_CQ, O_CKV, O_KR, O_ZD = 0, 512, 640, 768, 896, 1024, 1152, 1280, 1304, 1816, 2072, 2200, 2232


def phase_D(kb, c, tmp, fin, d, first, heads):
    NCH = 2 * NSB
    NK = NCH * 512
    nh = len(heads)
    npair = nh // 2
    Kn, r_Kn = kb.sb("D_Kn", [128, npair, NK], BF16)
    Kr, r_Kr = kb.sb("D_Kr", [32, NK], BF16)
    Cl, r_Cl = kb.sb("D_Cl", [128, NK // 128, 128], BF16)
    wck, r_wck = kb.sb("D_wck", [128, 8, 160], BF16)
    load_w_cols(kb, wck, r_wck, 0, d['w_in'], O_CKV, 128, 'D_wck')
    load_w_cols(kb, wck, r_wck, 128, d['w_in'], O_KR, 32, 'D_wck')
    wukn, r_wukn = kb.sb("D_wukn", [128, npair, 128], BF16)
    wuv, r_wuv = kb.sb("D_wuv", [128, nh, 64], BF16)
    for hi, h in enumerate(heads):
        kb.dma(wukn[:, hi // 2, (hi % 2) * 64:(hi % 2) * 64 + 64], d['d_w_ukv'][:, h * 128:h * 128 + 64], writes=[r_wukn], q='pool', sem='D_wukn')
        kb.dma(wuv[:, hi, :], d['d_w_ukv'][:, h * 128 + 64:h * 128 + 128], writes=[r_wuv], q='pool', sem='D_wuv')
    wuqn, r_wuqn = kb.sb("D_wuqn", [128, 2, nh, 128], BF16)
    kb.memset('pool', wuqn[:], 0.0, [r_wuqn])
    wuqr, r_wuqr = kb.sb("D_wuqr", [128, 2, nh, 32], BF16)
    for hi, h in enumerate(heads):
        for kk in range(2):
            off = (hi % 2) * 64
            kb.dma(wuqn[:, kk, hi, off:off + 64], d['d_w_uq'][kk * 128:(kk + 1) * 128, h * 96:h * 96 + 64], writes=[r_wuqn], q='pool', sem='D_wuqn')
            kb.dma(wuqr[:, kk, hi, :], d['d_w_uq'][kk * 128:(kk + 1) * 128, h * 96 + 64:h * 96 + 96], writes=[r_wuqr], q='pool', sem='D_wuqr')
    wo, r_wo = kb.sb("D_wo", [64, nh, 1024], BF16)
    for hi, h in enumerate(heads):
        kb.dma(wo[:, hi, :], d['w_out'][512 + h * 64:512 + (h + 1) * 64, :], writes=[r_wo], q='pool', sem='D_wo')
    gT, r_gT = load_gain_T(kb, "D_gT", d['norm'])
    glkv, r_glkv = load_col(kb, "D_glkv", d['d_kv_lat_norm'], 128, 1)
    glq, r_glq = kb.sb("D_glq", [128, 2], F32)
    kb.dma(glq[:], d['d_q_lat_norm'].rearrange("(k p) -> p k", p=128), writes=[r_glq], nonc=True)
    gqn, r_gqn = load_col(kb, "D_gqn", d['d_nope_norm'][0, :], 64, 2)
    gkn, r_gkn = load_col(kb, "D_gkn", d['d_nope_norm'][1, :], 64, 2)
    gqr, r_gqr = load_col(kb, "D_gqr", d['d_rope_norm'][0, :], 32, 1)
    gkr, r_gkr = load_col(kb, "D_gkr", d['d_rope_norm'][1, :], 32, 1)
    xnT, r_xnT = kb.sb("D_xnT", [128, 8, 512], BF16)
    cs, r_cs = kb.sb("D_cs", [32, 2, 512], F32)
    cn, r_cn = kb.sb("D_cn", [128, 2, 512], BF16)
    ring = WRing(kb, "D_wr", 3, 128)
    bT, r_bT = c['bankT']
    norm64 = (c['blk'], 1.0)
    for ch in range(NCH):
        for j in range(4):
            norm_transpose_tile(kb, c, d['xb'][ch * 512 + j * 128:ch * 512 + (j + 1) * 128, :], gT, r_gT, xnT, r_xnT, j * 128)
        kb.dma(cs[:], d['cs32_full'][:, :, ch * 512:(ch + 1) * 512], writes=[r_cs], sem='D_cs')
        bk, r_bk = c['bank'][0]
        proj_fm(kb, bk, r_bk, wck, r_wck, 0, 128, xnT, r_xnT, 0, 512)
        hnr(kb, c, bk, r_bk, 512, 128, glkv, r_glkv, cn[:, 0, :], r_cn, tmp, norm=(c['ones'], 1.0 / 128))
        for j in range(4):
            kb.S.op('pe', lambda h, j=j: h.transpose(out=bT[:, j, :], in_=cn[:, 0, j * 128:(j + 1) * 128], identity=c['ident']),
                    reads=[r_cn, c['r_c']], writes=[r_bT])
        kb.copy('act', Cl[:, ch * 4:(ch + 1) * 4, :], bT[:, 0:4, :], [r_bT], [r_Cl])
        for pr in range(npair):
            bk, r_bk = c['bank'][pr % 2]
            kb.mm(bk[:, :], wukn[:, pr, :], cn[:, 0, :], True, True, [r_wukn, r_cn], [r_bk])
            hnr(kb, c, bk, r_bk, 512, 128, gkn, r_gkn, Kn[:, pr, ch * 512:(ch + 1) * 512], r_Kn, tmp, norm=norm64)
        bk, r_bk = c['bank'][1]
        proj_fm(kb, bk, r_bk, wck, r_wck, 128, 32, xnT, r_xnT, 0, 512)
        hnr(kb, c, bk, r_bk, 512, 32, gkr, r_gkr, Kr[:, ch * 512:(ch + 1) * 512], r_Kr, tmp, norm=(c['ones'][0:32, 0:32], 1.0 / 32),
            rope=(c['rot32'][0:32, 0:32], cs[:, 0, :], cs[:, 1, :], r_cs))
    Qn, r_Qn = kb.sb("D_Qn", [128, nh, 512], BF16)
    Qr, r_Qr = kb.sb("D_Qr", [32, nh, 512], BF16)
    zs, r_zs = kb.sb("D_zs", [64, nh, 512], BF16)
    mixD, r_mixD = kb.sb("D_mix", [64, nh, 512], BF16)
    dneg, r_dneg = kb.sb("D_neg", [128, 4, 1024], BF16)
    kb.dma(dneg[:], d['A_caus'].rearrange("i t s -> t i s"), writes=[r_dneg], q='pool', sem='D_neg')
    PT = [kb.sb("D_PT%d" % i, [128, 512], BF16) for i in range(2)]
    olsb, r_olsb = kb.sb("D_ol", [128, 512], BF16)
    pv, r_pv = kb.sb("D_prev", [128, 1024], F32)
    osb, r_osb = kb.sb("D_osb", [128, 1024], F32)
    sq2, r_sq2 = tmp['sq2']
    sq, r_sq = tmp['sq']
    rs, r_rs = tmp['rs']
    r_outd = Res("outdD")
    npt = 0
    SC = 96 ** -0.5
    for m in range(NSB):
        nkeys = (2 * m + 2) * 512
        for j in range(4):
            norm_transpose_tile(kb, c, d['xq'][m * 512 + j * 128:m * 512 + (j + 1) * 128, :], gT, r_gT, xnT, r_xnT, j * 128)
        kb.dma(cs[:], d['cs32_q'][m], writes=[r_cs], sem='D_cs')
        b0, r_b0 = c['bank'][0]
        b1, r_b1 = c['bank'][1]
        b2, r_b2 = c['bank'][2]
        w0, r_w0 = ring.load(d['w_in'], O_CQ, 128)
        proj_fm(kb, b0, r_b0, w0, r_w0, 0, 128, xnT, r_xnT, 0, 512)
        w1, r_w1 = ring.load(d['w_in'], O_CQ + 128, 128)
        proj_fm(kb, b1, r_b1, w1, r_w1, 0, 128, xnT, r_xnT, 0, 512)
        kb.act(sq[:, :], b0[:, :], AF.Square, [r_b0], [r_sq])
        kb.act(sq2[:, :], b1[:, :], AF.Square, [r_b1], [r_sq2])
        kb.mm(b2[:, :], c['ones'], sq[:, :], True, False, [c['r_c'], r_sq], [r_b2])
        kb.mm(b2[:, :], c['ones'], sq2[:, :], False, True, [c['r_c'], r_sq2], [r_b2])
        kb.act(rs[:, :], b2[:, :], AF.Ln, [r_b2], [r_rs], bias=tmp['eps'][:, 0:1], scale=1.0 / 256)
        kb.act(rs[:, :], rs[:, :], AF.Exp, [r_rs], [r_rs], scale=-0.5)
        kb.stt(cn[:, 0, :], b0[:, :], glq[:, 0:1], rs[:, :], ALU.mult, ALU.mult, [r_b0, r_glq, r_rs], [r_cn])
        kb.stt(cn[:, 1, :], b1[:, :], glq[:, 1:2], rs[:, :], ALU.mult, ALU.mult, [r_b1, r_glq, r_rs], [r_cn])
        for hi, h in enumerate(heads):
            bk, r_bk = c['bank'][hi % 2]
            for kk in range(2):
                kb.mm(bk[:, :], wuqn[:, kk, hi, :], cn[:, kk, :], kk == 0, kk == 1, [r_wuqn, r_cn], [r_bk])
            hnr(kb, c, bk, r_bk, 512, 128, gqn, r_gqn, Qn[:, hi, :], r_Qn, tmp, norm=norm64)
            for kk in range(2):
                kb.mm(bk[0:32, :], wuqr[:, kk, hi, :], cn[:, kk, :], kk == 0, kk == 1, [r_wuqr, r_cn], [r_bk])
            hnr(kb, c, bk, r_bk, 512, 32, gqr, r_gqr, Qr[:, hi, :], r_Qr, tmp, norm=(c['ones'][0:32, 0:32], 1.0 / 32),
                rope=(c['rot32'][0:32, 0:32], cs[:, 0, :], cs[:, 1, :], r_cs))
            wz, r_wz = ring.load(d['w_in'], O_ZD + h * 64, 64)
            proj_fm(kb, bk, r_bk, wz, r_wz, 0, 64, xnT, r_xnT, 0, 512)
            kb.act(zs[:, hi, :], bk[0:64, :], AF.Silu, [r_bk], [r_zs])
        nkt = nkeys // 128
        for hi, h in enumerate(heads):
            ol, r_ol = c['bank'][5]
            dn, r_dn = c['bank'][6]
            for kt in range(nkt):
                sbk, r_sbk = c['bank'][3 + (kt % 2)]
                masked = kt >= nkt - 8
                if masked:
                    for i in range(4):
                        kb.mm(sbk[:, i * 128:(i + 1) * 128], dneg[:, i, (kt - (nkt - 8)) * 128:(kt - (nkt - 8) + 1) * 128], c['ident'],
                              i == 0, False, [r_dneg, c['r_c']], [r_sbk])
                kb.mm(sbk[:, :], Kn[:, hi // 2, kt * 128:(kt + 1) * 128], Qn[:, hi, :], not masked, False, [r_Kn, r_Qn], [r_sbk])
                kb.mm(sbk[:, :], Kr[:, kt * 128:(kt + 1) * 128], Qr[:, hi, :], False, True, [r_Kr, r_Qr], [r_sbk])
                pt, r_pt = PT[npt % 2]
                npt += 1
                kb.act(pt[:], sbk[:], AF.Exp, [r_sbk], [r_pt], scale=SC)
                kb.mm(ol[:, :], Cl[:, kt, :], pt[:], kt == 0, kt == nkt - 1, [r_Cl, r_pt], [r_ol])
                kb.mm(dn[0:64, :], c['ones'][:, 0:64], pt[:], kt == 0, kt == nkt - 1, [c['r_c'], r_pt], [r_dn])
            kb.copy('act', olsb[:], ol[:, :], [r_ol], [r_olsb])
            b0, r_b0 = c['bank'][0]
            kb.mm(b0[0:64, :], wuv[:, hi, :], olsb[:], True, True, [r_wuv, r_olsb], [r_b0])
            ln, r_ln = fin['ln']
            rec, r_rec = fin['rec']
            kb.act(ln[0:64, :], dn[0:64, :], AF.Ln, [r_dn], [r_ln])
            kb.act(rec[0:64, :], ln[0:64, :], AF.Exp, [r_ln], [r_rec], scale=-1.0)
            kb.tt('pool', rec[0:64, :], rec[0:64, :], zs[:, hi, :], ALU.mult, [r_rec, r_zs], [r_rec])
            kb.tt('dve', mixD[:, hi, :], b0[0:64, :], rec[0:64, :], ALU.mult, [r_b0, r_rec], [r_mixD])
        for i in range(4):
            row0 = m * 512 + i * 128
            src = d['xq'] if first else d['out']
            kb.dma(pv[:], src[row0:row0 + 128, :], reads=[r_outd] if not first else [], writes=[r_pv], sem='D_prev')
            out_proj(kb, c, mixD[:, :, i * 128:(i + 1) * 128], r_mixD, nh, wo, r_wo, pv, r_pv, osb, r_osb, d['out'][row0:row0 + 128, :], r_outd)
    return r_outd


def dense_attn(kb, c, PT, npt, negm, r_negm, KT, r_KT, V, r_V, vbase, QT, r_QT, g, qs, ktiles, ob, r_ob, mask_all=True, mask_from=0):
    n = len(ktiles)
    for idx, kt in enumerate(ktiles):
        sbk, r_sbk = c['bank'][3 + (idx % 2)]
        use_mask = mask_all or idx >= mask_from
        if use_mask:
            kb.mm(sbk[:, :], negm[:, kt * 128:(kt + 1) * 128], c['i4'][:].rearrange("p a t -> p (a t)"), True, False, [r_negm, c['r_i4']], [r_sbk])
        for hh in range(4):
            kb.mm(sbk[:, hh * 128:(hh + 1) * 128], KT[:, kt * 128:(kt + 1) * 128], QT[:, 4 * g + hh, qs], (not use_mask) and hh == 0, hh == 3,
                  [r_KT, r_QT], [r_sbk])
        pt, r_pt = PT[npt[0] % 2]
        npt[0] += 1
        kb.act(pt[:], sbk[:], AF.Exp, [r_sbk], [r_pt], scale=0.125)
        kb.mm(ob[:, :], V[:, vbase + kt, g, :], pt[:], idx == 0, idx == n - 1, [r_V, r_pt], [r_ob])


def branch_finalize(kb, fin, ob, r_ob, gz_ap, r_gz, acc, r_acc, firstb, clamp):
    ln, r_ln = fin['ln']
    rec, r_rec = fin['rec']
    if clamp:
        kb.ts('dve', ln[64:128, :], ob[64:128, :], 1e-30, None, ALU.max, None, [r_ob], [r_ln])
        kb.act(ln[64:128, :], ln[64:128, :], AF.Ln, [r_ln], [r_ln])
    else:
        kb.act(ln[64:128, :], ob[64:128, :], AF.Ln, [r_ob], [r_ln])
    kb.act(rec[0:64, :], ln[64:128, :], AF.Exp, [r_ln], [r_rec], scale=-1.0)
    kb.tt('pool', rec[0:64, :], rec[0:64, :], gz_ap, ALU.mult, [r_rec, r_gz], [r_rec])
    if firstb:
        kb.tt('dve', acc[:, :], ob[0:64, :], rec[0:64, :], ALU.mult, [r_ob, r_rec], [r_acc])
    else:
        kb.tt('dve', ln[0:64, :], ob[0:64, :], rec[0:64, :], ALU.mult, [r_ob, r_rec], [r_ln])
        kb.tt('pool', acc[:, :], acc[:, :], ln[0:64, :], ALU.add, [r_acc, r_ln], [r_acc])


def phase_C(kb, c, tmp, fin, d, first, branches=('c', 's', 'w')):
    NCH = 2 * NSB
    NK = NCH * 512
    NSL = NCH * 32
    NCT = (NSL + 127) // 128
    norm64 = (c['blk'], 1.0)
    rot64 = c['rot']
    KsT, r_KsT = kb.sb("C_KsT", [128, NK], BF16)
    Vs, r_Vs = kb.sb("C_Vs", [128, NK // 128, 2, 128], BF16)
    kb.memset('pool', Vs[:, :, :, 64:128], 1.0, [r_Vs])
    KcT, r_KcT = kb.sb("C_KcT", [128, 2, NCT * 128], BF16)
    kb.memset('pool', KcT[:], 0.0, [r_KcT])
    Vc, r_Vc = kb.sb("C_Vc", [128, NCT, 2, 128], BF16)
    kb.memset('pool', Vc[:, :, :, 0:64], 0.0, [r_Vc])
    kb.memset('pool', Vc[:, :, :, 64:128], 1.0, [r_Vc])
    wo, r_wo = kb.sb("C_wo", [64, 8, 1024], BF16)
    kb.dma(wo[:], d['w_out'][0:512, :].rearrange("(h d) n -> d h n", d=64), writes=[r_wo], q='pool', sem='C_wo')
    gT, r_gT = load_gain_T(kb, "C_gT", d['norm'])
    gq, r_gq = load_col(kb, "C_gq", d['c_q_norm'], 64, 2)
    gkc, r_gkc = load_col(kb, "C_gkc", d['c_k_norm'][0, :], 64, 2)
    gks, r_gks = load_col(kb, "C_gks", d['c_k_norm'][1, :], 64, 2)
    gkw, r_gkw = load_col(kb, "C_gkw", d['c_k_norm'][2, :], 64, 2)
    xnT, r_xnT = kb.sb("C_xnT", [128, 8, 512], BF16)
    cs, r_cs = kb.sb("C_cs", [128, 2, 512], F32)
    ring = WRing(kb, "C_wr", 3, 128)
    ringL = WRing(kb, "C_wl", 2, 128, zero=True)
    ringR = WRing(kb, "C_wR", 2, 128, zero=True)
    kb.push()
    wk, r_wk = kb.sb("C_wk", [128, 8, 512], BF16)
    load_w_cols(kb, wk, r_wk, 0, d['w_in'], O_KS, 128, 'C_wk')
    load_w_cols(kb, wk, r_wk, 128, d['w_in'], O_VS, 128, 'C_wk')
    load_w_cols(kb, wk, r_wk, 256, d['w_in'], O_KC, 128, 'C_wk')
    load_w_cols(kb, wk, r_wk, 384, d['w_in'], O_VC, 128, 'C_wk')
    w1, r_w1 = kb.sb("C_w1", [64, 2, 32, 128], BF16)
    for kv in range(2):
        kb.dma(w1[:, kv, :, :], d['c_cmp_w1'][kv].rearrange("(l dd) h -> dd l h", dd=64), writes=[r_w1], q='pool', sem='C_w1')
    w2p, r_w2p = kb.sb("C_w2p", [128, 2, 128], BF16)
    kb.memset('pool', w2p[:], 0.0, [r_w2p])
    for g in range(2):
        kb.dma(w2p[:, g, g * 64:(g + 1) * 64], d['c_cmp_w2'][0], writes=[r_w2p], q='pool', sem='C_w2p')
    w2v, r_w2v = kb.sb("C_w2v", [128, 64], BF16)
    kb.dma(w2v[:], d['c_cmp_w2'][1], writes=[r_w2v], q='pool', sem='C_w2v')
    peT, r_peT = kb.sb("C_peT", [64, 2, 32], F32)
    for kv in range(2):
        kb.dma(peT[:, kv, :], d['c_cmp_pe'][kv].rearrange("l dd -> dd l"), writes=[r_peT], nonc=True, sem='C_peT')
    peb, r_peb = kb.sb("C_peb", [64, 2, 32], BF16)
    kb.copy('dve', peb[:], peT[:], [r_peT], [r_peb])
    cb_, r_cb_ = kb.sb("C_cbias", [128, 2], F32)
    for kv in range(2):
        bk, r_bk = c['bank'][0]
        for l in range(32):
            kb.mm(bk[:, kv:kv + 1], w1[:, kv, l, :], peb[:, kv, l:l + 1], l == 0, l == 31, [r_w1, r_peb], [r_bk])
        kb.copy('dve', cb_[:, kv:kv + 1], bk[:, kv:kv + 1], [r_bk], [r_cb_])
    raw, r_raw = kb.sb("C_raw", [64, 2, 2, 528], BF16)
    kb.memset('pool', raw[:], 0.0, [r_raw])
    hsb, r_hsb = kb.sb("C_hsb", [128, 32], BF16)
    ccs, r_ccs = kb.sb("C_ccs", [128, 2, NCT * 128], F32)
    kb.dma(ccs[:, :, 0:NSL], d['ccs'][:, :, 0:NSL], writes=[r_ccs])
    for ch in range(NCH):
        for j in range(4):
            norm_transpose_tile(kb, c, d['xb'][ch * 512 + j * 128:ch * 512 + (j + 1) * 128, :], gT, r_gT, xnT, r_xnT, j * 128)
        kb.dma(cs[:], d['cs_full'][:, :, ch * 512:(ch + 1) * 512], writes=[r_cs], sem='C_cs')
        bk, r_bk = c['bank'][0]
        proj_fm(kb, bk, r_bk, wk, r_wk, 0, 128, xnT, r_xnT, 0, 512)
        hnr(kb, c, bk, r_bk, 512, 128, gks, r_gks, KsT[:, ch * 512:(ch + 1) * 512], r_KsT, tmp, norm=norm64, rope=(rot64, cs[:, 0, :], cs[:, 1, :], r_cs))
        for j in range(4):
            bk, r_bk = c['bank'][j % 2]
            for k in range(8):
                kb.mm(bk[:, 0:128], xnT[:, k, j * 128:(j + 1) * 128], wk[:, k, 128:256], k == 0, k == 7, [r_xnT, r_wk], [r_bk])
            kb.copy('act', Vs[:, ch * 4 + j, :, 0:64], bk[:, 0:128].rearrange("p (g d) -> p g d", g=2), [r_bk], [r_Vs])
        if ch > 0:
            kb.copy('pool', raw[:, :, :, 0:16], raw[:, :, :, 512:528], [r_raw], [r_raw])
        for kv in range(2):
            for g in range(2):
                bk, r_bk = c['bank'][g]
                proj_fm(kb, bk, r_bk, wk, r_wk, 256 + kv * 128 + g * 64, 64, xnT, r_xnT, 0, 512)
                kb.copy('act', raw[:, kv, g, 16:528], bk[0:64, :], [r_bk], [r_raw])
        for kv in range(2):
            for g in range(2):
                bk, r_bk = c['bank'][g]
                for l in range(32):
                    kb.mm(bk[:, 0:32], w1[:, kv, l, :], raw[:, kv, g, l:l + 497:16], l == 0, l == 31, [r_w1, r_raw], [r_bk])
                kb.act(hsb[:, :], bk[:, 0:32], AF.Silu, [r_bk, r_cb_], [r_hsb], bias=cb_[:, kv:kv + 1])
                b1, r_b1 = c['bank'][4]
                if kv == 0:
                    kb.mm(b1[:, 0:32], w2p[:, g, :], hsb[:, :], True, True, [r_w2p, r_hsb], [r_b1])
                    hnr(kb, c, b1, r_b1, 32, 128, gkc, r_gkc, KcT[:, g, ch * 32:(ch + 1) * 32], r_KcT, tmp, norm=norm64,
                        rope=(rot64, ccs[:, 0, ch * 32:(ch + 1) * 32], ccs[:, 1, ch * 32:(ch + 1) * 32], r_ccs))
                else:
                    kb.mm(b1[0:32, 0:64], hsb[:, :], w2v[:, :], True, True, [r_hsb, r_w2v], [r_b1])
                    po = 32 * (ch % 4)
                    kb.copy('dve', Vc[po:po + 32, ch // 4, g, 0:64], b1[0:32, 0:64], [r_b1], [r_Vc])
    kb.pop()
    QT, r_QT = kb.sb("C_QT", [128, 8, 512], BF16)
    zs, r_zs = kb.sb("C_zs", [64, 8, 512], BF16)
    gsb, r_gsb = kb.sb("C_gsb", [128, 4, 24], F32)
    dg, r_dg = kb.sb("C_dg", [128, 128], BF16)
    gz, r_gz = kb.sb("C_gz", [64, 6, 512], BF16)
    KwT, r_KwT = kb.sb("C_KwT", [128, 1024], BF16)
    Vw, r_Vw = kb.sb("C_Vw", [128, 8, 2, 128], BF16)
    kb.memset('pool', Vw[:, :, :, 64:128], 1.0, [r_Vw])
    negm, r_negm = kb.sb("C_negm", [128, NK], BF16)
    dneg, r_dneg = kb.sb("C_dneg", [128, 4, 1024], BF16)
    kb.dma(dneg[:], d['A_caus'].rearrange("i t s -> t i s"), writes=[r_dneg], q='pool', sem='C_dneg')
    cmask, r_cmask = kb.sb("C_cmask", [128, NCT * 128], BF16)
    wmask, r_wmask = kb.sb("C_wmask", [128, 640], BF16)
    wsel, r_wsel = kb.sb("C_wsel", [128, NCT, 128], BF16)
    kb.dma(wsel[:], d['C_wsel'][0:NCT * 128].rearrange("(j p) b -> p j b", p=128), writes=[r_wsel], q='pool', sem='C_wsel')
    seladd, r_seladd = kb.sb("C_seladd", [128, 128], F32)
    imp, r_imp = kb.sb("C_imp", [128, 128], F32)
    imp2, r_imp2 = kb.sb("C_imp2", [128, 128], F32)
    m8, r_m8 = kb.sb("C_m8", [128, 24], F32)
    negblk, r_negblk = kb.sb("C_negblk", [128, 128], BF16)
    acc, r_acc = kb.sb("C_acc", [64, 512], F32)
    mix, r_mix = kb.sb("C_mix", [64, 8, 128], BF16)
    PT = [kb.sb("C_PT%d" % i, [128, 512], BF16) for i in range(2)]
    pv, r_pv = kb.sb("C_prev", [128, 1024], F32)
    osb, r_osb = kb.sb("C_osb", [128, 1024], F32)
    wg, r_wg = kb.sb("C_wg", [128, 8, 24], BF16)
    load_w_cols(kb, wg, r_wg, 0, d['w_in'], O_GC, 24, 'C_wg')
    r_outd = Res("outdC")
    npt = [0]
    for m in range(NSB):
        nkeys = (2 * m + 2) * 512
        nct = ((2 * m + 2) * 32 + 127) // 128
        for cc, src in enumerate((d['xh'][m], d['xq'][m * 512:(m + 1) * 512, :])):
            for j in range(4):
                norm_transpose_tile(kb, c, src[j * 128:(j + 1) * 128, :], gT, r_gT, xnT, r_xnT, j * 128)
            kb.dma(cs[:], d['cs_w'][m, :, :, cc * 512:(cc + 1) * 512], writes=[r_cs], sem='C_cs')
            wt_, r_wt = ring.load(d['w_in'], O_KW, 128)
            bk, r_bk = c['bank'][0]
            proj_fm(kb, bk, r_bk, wt_, r_wt, 0, 128, xnT, r_xnT, 0, 512)
            hnr(kb, c, bk, r_bk, 512, 128, gkw, r_gkw, KwT[:, cc * 512:(cc + 1) * 512], r_KwT, tmp, norm=norm64, rope=(rot64, cs[:, 0, :], cs[:, 1, :], r_cs))
            wt_, r_wt = ring.load(d['w_in'], O_VW, 128)
            for j in range(4):
                bk, r_bk = c['bank'][j % 2]
                for k in range(8):
                    kb.mm(bk[:, 0:128], xnT[:, k, j * 128:(j + 1) * 128], wt_[:, k, 0:128], k == 0, k == 7, [r_xnT, r_wt], [r_bk])
                kb.copy('act', Vw[:, cc * 4 + j, :, 0:64], bk[:, 0:128].rearrange("p (g d) -> p g d", g=2), [r_bk], [r_Vw])
        for h in range(8):
            rg = ringL if h < 4 else ringR
            wt_, r_wt = rg.load(d['w_in'], O_QC + h * 64, 64, dst=0 if h < 4 else 64)
            bk, r_bk = c['bank'][h % 2]
            proj_fm(kb, bk, r_bk, wt_, r_wt, 0, 128, xnT, r_xnT, 0, 512)
            hnr(kb, c, bk, r_bk, 512, 128, gq, r_gq, QT[:, h, :], r_QT, tmp, norm=norm64, rope=(rot64, cs[:, 0, :], cs[:, 1, :], r_cs))
        for h in range(8):
            wt_, r_wt = ring.load(d['w_in'], O_ZC + h * 64, 64)
            bk, r_bk = c['bank'][h % 2]
            proj_fm(kb, bk, r_bk, wt_, r_wt, 0, 64, xnT, r_xnT, 0, 512)
            kb.act(zs[:, h, :], bk[0:64, :], AF.Silu, [r_bk], [r_zs])
        for i in range(4):
            bk, r_bk = c['bank'][i % 2]
            for k in range(8):
                kb.mm(bk[:, 0:24], xnT[:, k, i * 128:(i + 1) * 128], wg[:, k, 0:24], k == 0, k == 7, [r_xnT, r_wg], [r_bk])
            kb.act(gsb[:, i, :], bk[:, 0:24], AF.Sigmoid, [r_bk], [r_gsb])
        for i in range(4):
            qs = slice(i * 128, (i + 1) * 128)
            for g in range(2):
                for br in range(3):
                    bk, r_bk = c['bank'][(g * 3 + br) % 2]
                    for hh in range(4):
                        col = (4 * g + hh) * 3 + br
                        kb.ts('pool', dg[:, :], c['ident'], gsb[:, i, col:col + 1], None, ALU.mult, None, [c['r_c'], r_gsb], [r_dg])
                        kb.mm(bk[0:64, hh * 128:(hh + 1) * 128], c['ones'][:, 0:64], dg[:, :], hh == 0, hh == 3, [c['r_c'], r_dg], [r_bk])
                    kb.tt('dve', gz[:, g * 3 + br, :].rearrange("p (h t) -> p h t", h=4), bk[0:64, :].rearrange("p (h t) -> p h t", h=4),
                          zs[:, 4 * g:4 * g + 4, qs], ALU.mult, [r_bk, r_zs], [r_gz])
            if 'c' in branches or 's' in branches:
                kb.dma(cmask[:, 0:nct * 128], d['C_cmask'][m, i, :, 0:nct * 128], writes=[r_cmask], q='pool', sem='C_cmask')
            if 's' in branches:
                kb.dma(seladd[:], d['C_seladd'][m, i], writes=[r_seladd], sem='C_seladd')
            if 'w' in branches:
                kb.dma(wmask[:], d['W_masks'][0 if m == 0 else 1, i], writes=[r_wmask], q='pool', sem='C_wmask')
            for g in range(2):
                ob, r_ob = c['bank'][5]
                firstb = True
                if 'c' in branches or 's' in branches:
                    U, r_U = c['bank'][0]
                    dq, r_dq = c['bank'][1]
                    for j in range(nct):
                        sbk, r_sbk = c['bank'][3 + (j % 2)]
                        kb.mm(sbk[:, :], cmask[:, j * 128:(j + 1) * 128], c['i4'][:].rearrange("p a t -> p (a t)"), True, False, [r_cmask, c['r_i4']], [r_sbk])
                        for hh in range(4):
                            kb.mm(sbk[:, hh * 128:(hh + 1) * 128], KcT[:, g, j * 128:(j + 1) * 128], QT[:, 4 * g + hh, qs], False, hh == 3, [r_KcT, r_QT], [r_sbk])
                        pt, r_pt = PT[npt[0] % 2]
                        npt[0] += 1
                        kb.act(pt[:], sbk[:], AF.Exp, [r_sbk], [r_pt], scale=0.125)
                        kb.mm(ob[:, :], Vc[:, j, g, :], pt[:], j == 0, j == nct - 1, [r_Vc, r_pt], [r_ob])
                        if 's' in branches:
                            for hh in range(4):
                                kb.mm(U[:, hh * 128:(hh + 1) * 128], pt[:, hh * 128:(hh + 1) * 128], wsel[:, j, :], j == 0 and hh == 0, j == nct - 1 and hh == 3,
                                      [r_pt, r_wsel], [r_U])
                            for hh in range(4):
                                kb.mm(dq[:, hh:hh + 1], pt[:, hh * 128:(hh + 1) * 128], c['ones'][:, 0:1], j == 0 and hh == 0, j == nct - 1 and hh == 3,
                                      [r_pt, c['r_c']], [r_dq])
                    if 'c' in branches:
                        branch_finalize(kb, fin, ob, r_ob, gz[:, g * 3 + 0, :], r_gz, acc, r_acc, firstb, True)
                        firstb = False
                if 's' in branches:
                    kb.ts('dve', m8[:, 16:20], dq[:, 0:4], 1e-30, None, ALU.max, None, [r_dq], [r_m8])
                    kb.S.op('dve', lambda h: h.reciprocal(out=m8[:, 20:24], in_=m8[:, 16:20]), reads=[r_m8], writes=[r_m8])
                    for hh in range(4):
                        if hh == 0:
                            kb.ts('dve', imp[:, :], U[:, 0:128], m8[:, 20:21], None, ALU.mult, None, [r_U, r_m8], [r_imp])
                        else:
                            kb.stt(imp[:, :], U[:, hh * 128:(hh + 1) * 128], m8[:, 20 + hh:21 + hh], imp[:, :], ALU.mult, ALU.add, [r_U, r_m8, r_imp], [r_imp])
                    kb.tt('dve', imp[:, :], imp[:, :], seladd[:, :], ALU.add, [r_imp, r_seladd], [r_imp])
                    kb.S.op('dve', lambda h: h.max(out=m8[:, 0:8], in_=imp[:, :]), reads=[r_imp], writes=[r_m8])
                    kb.S.op('dve', lambda h: h.match_replace(out=imp2[:, :], in_to_replace=m8[:, 0:8], in_values=imp[:, :], imm_value=-3.0e38),
                            reads=[r_imp, r_m8], writes=[r_imp2])
                    kb.S.op('dve', lambda h: h.max(out=m8[:, 8:16], in_=imp2[:, :]), reads=[r_imp2], writes=[r_m8])
                    kb.ts('dve', m8[:, 15:16], m8[:, 15:16], -1.0e29, None, ALU.max, None, [r_m8], [r_m8])
                    kb.ts('dve', negblk[:, :], imp[:, :], m8[:, 15:16], NEG, ALU.is_lt, ALU.mult, [r_imp, r_m8], [r_negblk])
                    nb = nkeys // 64
                    kb.copy('dve', negm[:, 0:nkeys].rearrange("p (b s) -> p b s", s=64), negblk[:, 0:nb].unsqueeze(2).to_broadcast([128, nb, 64]),
                            [r_negblk], [r_negm])
                    kb.tt('dve', negm[:, nkeys - 1024:nkeys], negm[:, nkeys - 1024:nkeys], dneg[:, i, :], ALU.add, [r_negm, r_dneg], [r_negm])
                    dense_attn(kb, c, PT, npt, negm, r_negm, KsT, r_KsT, Vs, r_Vs, 0, QT, r_QT, g, qs, list(range(nkeys // 128)), ob, r_ob)
                    branch_finalize(kb, fin, ob, r_ob, gz[:, g * 3 + 1, :], r_gz, acc, r_acc, firstb, False)
                    firstb = False
                if 'w' in branches:
                    for idx in range(5):
                        kt = i + idx
                        sbk, r_sbk = c['bank'][3 + (idx % 2)]
                        kb.mm(sbk[:, :], wmask[:, idx * 128:(idx + 1) * 128], c['i4'][:].rearrange("p a t -> p (a t)"), True, False, [r_wmask, c['r_i4']], [r_sbk])
                        for hh in range(4):
                            kb.mm(sbk[:, hh * 128:(hh + 1) * 128], KwT[:, kt * 128:(kt + 1) * 128], QT[:, 4 * g + hh, qs], False, hh == 3, [r_KwT, r_QT], [r_sbk])
                        pt, r_pt = PT[npt[0] % 2]
                        npt[0] += 1
                        kb.act(pt[:], sbk[:], AF.Exp, [r_sbk], [r_pt], scale=0.125)
                        kb.mm(ob[:, :], Vw[:, kt, g, :], pt[:], idx == 0, idx == 4, [r_Vw, r_pt], [r_ob])
                    branch_finalize(kb, fin, ob, r_ob, gz[:, g * 3 + 2, :], r_gz, acc, r_acc, firstb, False)
                    firstb = False
                kb.copy('act', mix[:, 4 * g:4 * g + 4, :], acc[:, :].rearrange("p (h t) -> p h t", h=4), [r_acc], [r_mix])
            row0 = m * 512 + i * 128
            src = d['xq'] if first else d['out']
            kb.dma(pv[:], src[row0:row0 + 128, :], reads=[r_outd] if not first else [], writes=[r_pv], sem='C_prev')
            out_proj(kb, c, mix, r_mix, 8, wo, r_wo, pv, r_pv, osb, r_osb, d['out'][row0:row0 + 128, :], r_outd)
    return r_outd


def build_layer1(phases=('D0', 'D1')):
    kb = KB()
    d = {}
    d['xb'] = kb.din("xb", [S_, 1024])
    d['xq'] = kb.din("xq", [NSB * SB, 1024])
    d['w_in'] = kb.din("w_in", [1024, 2744])
    d['w_out'] = kb.din("w_out", [1024, 1024])
    d['norm'] = kb.din("norm", [1024])
    d['d_q_lat_norm'] = kb.din("d_q_lat_norm", [256])
    d['d_kv_lat_norm'] = kb.din("d_kv_lat_norm", [128])
    d['d_w_uq'] = kb.din("d_w_uq", [256, 768])
    d['d_w_ukv'] = kb.din("d_w_ukv", [128, 1024])
    d['d_nope_norm'] = kb.din("d_nope_norm", [2, 64])
    d['d_rope_norm'] = kb.din("d_rope_norm", [2, 32])
    d['cs32_full'] = kb.din("cs32_full", [32, 2, S_])
    d['cs32_q'] = kb.din("cs32_q", [NSB, 32, 2, 512])
    d['A_caus'] = kb.din("A_caus", [4, 128, 1024])
    d['c_q_norm'] = kb.din("c_q_norm", [64])
    d['c_k_norm'] = kb.din("c_k_norm", [3, 64])
    d['c_cmp_pe'] = kb.din("c_cmp_pe", [2, 32, 64])
    d['c_cmp_w1'] = kb.din("c_cmp_w1", [2, 2048, 128])
    d['c_cmp_w2'] = kb.din("c_cmp_w2", [2, 128, 64])
    d['cs_full'] = kb.din("cs_full", [128, 2, S_])
    d['ccs'] = kb.din("ccs", [128, 2, 512])
    d['xh'] = kb.din("xh", [NSB, 512, 1024])
    d['cs_w'] = kb.din("cs_w", [NSB, 128, 2, 1024])
    d['C_cmask'] = kb.din("C_cmask", [NSB, 4, 128, 512])
    d['C_seladd'] = kb.din("C_seladd", [NSB, 4, 128, 128])
    d['W_masks'] = kb.din("W_masks", [2, 4, 128, 640])
    d['C_wsel'] = kb.din("C_wsel", [512, 128])
    d['out'] = kb.dout("out", [NSB * SB, 1024])
    with kb.es:
        kb.start()
        c = common_setup(kb)
        tmp = make_tmp(kb)
        fin = {'ln': kb.sb("f_ln", [128, 512], F32), 'rec': kb.sb("f_rec", [128, 512], F32)}
        finals = []
        first = True
        for ph in phases:
            kb.push()
            if ph.startswith('C') and len(ph) > 1:
                finals.append(phase_C(kb, c, tmp, fin, d, first, tuple(ph[1:])))
            elif ph == 'D0':
                finals.append(phase_D(kb, c, tmp, fin, d, first, [0, 1, 2, 3]))
            elif ph == 'D1':
                finals.append(phase_D(kb, c, tmp, fin, d, first, [4, 5, 6, 7]))
            elif ph == 'C':
                finals.append(phase_C(kb, c, tmp, fin, d, first))
            kb.pop()
            first = False
        kb.S.finish(finals)
    return kb


def layer1_inputs(x1, p, core):
    b, half = core // 2, core % 2
    xb = np.ascontiguousarray(x1[b])
    sbs = [2 * m + half for m in range(NSB)]
    xq = np.concatenate([xb[s * 512:(s + 1) * 512] for s in sbs], 0)
    c32, s32 = rope_tab(np.arange(S_), 32)
    cs32_full = np.stack([np.tile(c32.T, (2, 1)), np.tile(s32.T, (2, 1))], 1)
    cs32_q = np.stack([cs32_full[:, :, s * 512:(s + 1) * 512] for s in sbs], 0)
    A_caus = np.zeros((4, 128, 1024), np.float32)
    for i in range(4):
        tq = half * 512 + i * 128 + np.arange(128)[:, None]
        A_caus[i] = np.where(np.arange(1024)[None, :] <= tq, 0.0, -1e30)
    im = {"xb": xb, "xq": xq, "w_in": p['cd_w_in'][0], "w_out": p['cd_w_out'][0], "norm": p['cd_norm'][0],
          "d_q_lat_norm": p['d_q_lat_norm'][0], "d_kv_lat_norm": p['d_kv_lat_norm'][0], "d_w_uq": p['d_w_uq'][0],
          "d_w_ukv": p['d_w_ukv'][0], "d_nope_norm": p['d_nope_norm'][0], "d_rope_norm": p['d_rope_norm'][0],
          "cs32_full": cs32_full, "cs32_q": cs32_q, "A_caus": A_caus}
    im.update(consts())
    im.update({"c_q_norm": p['c_q_norm'][0], "c_k_norm": p['c_k_norm'][0], "c_cmp_pe": p['c_cmp_pe'][0], "c_cmp_w1": p['c_cmp_w1'][0],
               "c_cmp_w2": p['c_cmp_w2'][0]})
    cosf, sinf = rope_tab(np.arange(S_), 64)
    im["cs_full"] = np.stack([np.tile(cosf.T, (4, 1)), np.tile(sinf.T, (4, 1))], 1)
    slot = np.arange(512)
    cc_, sc_ = rope_tab(np.maximum(16 * slot + 15, 0), 64)
    im["ccs"] = np.stack([np.tile(cc_.T, (4, 1)), np.tile(sc_.T, (4, 1))], 1)
    xh = np.zeros((NSB, 512, 1024), np.float32)
    cs_w = np.zeros((NSB, 128, 2, 1024), np.float32)
    C_cmask = np.zeros((NSB, 4, 128, 512), np.float32)
    C_seladd = np.zeros((NSB, 4, 128, 128), np.float32)
    for m, s_ in enumerate(sbs):
        lo = s_ * 512 - 512
        if lo >= 0:
            xh[m] = xb[lo:lo + 512]
        else:
            xh[m] = xb[0:512]
        pos = np.maximum(np.arange(lo, lo + 1024), 0)
        cw, sw = rope_tab(pos, 64)
        cs_w[m, :, 0, :] = np.tile(cw.T, (4, 1))
        cs_w[m, :, 1, :] = np.tile(sw.T, (4, 1))
        for i in range(4):
            t = (s_ * 512 + i * 128 + np.arange(128))[:, None]
            C_cmask[m, i] = np.where((slot[None, :] >= 1) & (16 * slot[None, :] + 15 <= t), 0.0, -1e30)
            bid = np.arange(128)[None, :]
            cur = t // 64
            admiss = bid <= cur
            forced = (bid == 0) | (bid >= cur - 1)
            C_seladd[m, i] = np.where(admiss & forced, 1e30, np.where(admiss, 0.0, -1e30))
    W_masks = np.zeros((2, 4, 128, 640), np.float32)
    for v in range(2):
        for i in range(4):
            tl = (512 + i * 128 + np.arange(128))[:, None]
            kl = (i * 128 + np.arange(640))[None, :]
            ok = (kl <= tl) & (tl - kl < 512)
            if v == 0 and half == 0:
                ok = ok & (kl >= 512)
            W_masks[v, i] = np.where(ok, 0.0, -1e30)
    wsel = np.zeros((512, 128), np.float32)
    for j in range(128):
        for n_, w_ in ((4 * j, 1.0), (4 * j + 1, 2.0), (4 * j + 2, 2.0), (4 * j + 3, 2.0), (4 * j + 4, 1.0)):
            if 1 <= n_ < 512:
                wsel[n_, j] = w_
    im.update({"xh": xh, "cs_w": cs_w, "C_cmask": C_cmask, "C_seladd": C_seladd, "W_masks": W_masks, "C_wsel": wsel})
    return im


def rope_tab(pos, dim):
    inv = np.power(np.float32(10000.0), -np.arange(0, dim, 2, dtype=np.float32) / np.float32(dim)).astype(np.float32)
    ang = (pos.astype(np.float32)[:, None] * inv[None, :]).astype(np.float32)
    return np.cos(ang.astype(np.float64)).astype(np.float32), np.sin(ang.astype(np.float64)).astype(np.float32)


def consts():
    ident = np.eye(128, dtype=np.float32)
    blk = np.zeros((128, 128), np.float32)
    blk[0:64, 0:64] = 1.0 / 64
    blk[64:128, 64:128] = 1.0 / 64
    rot = np.zeros((128, 128), np.float32)
    for mm_ in range(128):
        if mm_ % 64 < 32:
            rot[mm_ + 32, mm_] = -1.0
        else:
            rot[mm_ - 32, mm_] = 1.0
    rot32 = np.zeros((128, 128), np.float32)
    for mm_ in range(32):
        if mm_ < 16:
            rot32[mm_ + 16, mm_] = -1.0
        else:
            rot32[mm_ - 16, mm_] = 1.0
    return {"c_ident": ident, "c_blk64": blk, "c_rot64": rot, "c_ones": np.ones((128, 128), np.float32), "c_rot32": rot32}


def layer0_inputs(x, p, core):
    b, half = core // 2, core % 2
    xb = np.ascontiguousarray(x[b])
    sbs = [2 * m + half for m in range(NSB)]
    xq = np.concatenate([xb[s * 512:(s + 1) * 512] for s in sbs], 0)
    xq_halo = np.zeros((NSB, 640, 1024), np.float32)
    cs_halo = np.zeros((NSB, 128, 2, 640), np.float32)
    for m, s in enumerate(sbs):
        lo = s * 512 - 128
        pos = np.arange(lo, lo + 640)
        if lo >= 0:
            xq_halo[m] = xb[lo:lo + 640]
        else:
            xq_halo[m, 128:] = xb[0:512]
            xq_halo[m, :128] = xb[0:128]
        cos, sin = rope_tab(np.maximum(pos, 0), 64)
        cs_halo[m, :, 0, :] = np.tile(cos.T, (4, 1))
        cs_halo[m, :, 1, :] = np.tile(sin.T, (4, 1))
    s_i = np.arange(128)[:, None]
    t_i = np.arange(128)[None, :]
    prev = np.where(s_i > t_i, 0.0, NEG).astype(np.float32)
    diag = np.where(s_i <= t_i, 0.0, NEG).astype(np.float32)
    prev0 = prev if half == 1 else np.full((128, 128), NEG, np.float32)
    cosf, sinf = rope_tab(np.arange(S_), 64)
    cs_full = np.stack([np.tile(cosf.T, (4, 1)), np.tile(sinf.T, (4, 1))], 1)
    A_caus = np.zeros((4, 128, 1024), np.float32)
    for i in range(4):
        tq = half * 512 + i * 128 + np.arange(128)[:, None]
        A_caus[i] = np.where(np.arange(1024)[None, :] <= tq, 0.0, -1e30)
    im = {"a_qk_norm": p['a_qk_norm'][0], "a_kidx_norm": p['a_kidx_norm'][0], "cs_full": cs_full, "A_caus": A_caus,
          "xb": xb, "xq": xq, "xq_halo": xq_halo, "cs_halo": cs_halo,
          "w_in": p['ab_w_in'][0], "w_out": p['ab_w_out'][0], "norm": p['ab_norm'][0],
          "b_qk_norm": p['b_qk_norm'][0], "b_sinks": p['b_sinks'][0],
          "B_masks": np.stack([prev, diag, prev0])}
    im.update(consts())
    return im


def scatter_out(outs):
    full = np.zeros((4, S_, 1024), np.float32)
    for core, o in enumerate(outs):
        b, half = core // 2, core % 2
        for m in range(NSB):
            s = 2 * m + half
            full[b, s * 512:(s + 1) * 512] = o[m * 512:(m + 1) * 512]
    return full


_CACHE = {}


def run_layer0(x, p, phases):
    key = ('l0', tuple(phases))
    if key not in _CACHE:
        _CACHE[key] = build_layer0(phases)
    kb = _CACHE[key]
    in_maps = []
    for core in range(8):
        im = layer0_inputs(x, p, core)
        in_maps.append({k: np.ascontiguousarray(im[k], dtype=np.float32) for k in kb.inputs})
    res = run_bass_kernel_spmd(kb.nc, in_maps, core_ids=list(range(8)))
    return scatter_out([r["out"] for r in res.results])


def run_layer1(x1, p, phases):
    key = ('l1', tuple(phases))
    if key not in _CACHE:
        _CACHE[key] = build_layer1(phases)
    kb = _CACHE[key]
    in_maps = []
    for core in range(8):
        im = layer1_inputs(x1, p, core)
        in_maps.append({k: np.ascontiguousarray(im[k], dtype=np.float32) for k in kb.inputs})
    res = run_bass_kernel_spmd(kb.nc, in_maps, core_ids=list(range(8)))
    return scatter_out([r["out"] for r in res.results])


def kernel(**inputs):
    p = {k: np.asarray(v, dtype=np.float32) for k, v in inputs.items()}
    x = p['x']
    x1 = run_layer0(x, p, ('A', 'B'))
    out = run_layer1(x1, p, ('Ccsw', 'D0', 'D1'))
    return out.astype(np.float32)
```
